# Optimizing a Trainium2 kernel written in Bass

```python
import jax, jax.numpy as jnp
from jax import lax
import numpy as np

D_MODEL = 1024
BATCH = 16
SEQ = 2048
DEPTH = 1

MIX_WIDTH = D_MODEL
RET_HEADS = 4
RET_DK = MIX_WIDTH // (2 * RET_HEADS)
RET_DV = MIX_WIDTH // (2 * RET_HEADS)
RET_WIDTH = RET_HEADS * RET_DV
RET_CHUNK = 128
DIFF_HEADS = 4
DIFF_DQK = MIX_WIDTH // (4 * DIFF_HEADS)
DIFF_DV = 2 * DIFF_DQK
DIFF_WIDTH = DIFF_HEADS * DIFF_DV
Q_BLOCK = 128
IN_SPLITS = [RET_HEADS * RET_DK, RET_HEADS * RET_DK, RET_WIDTH, RET_WIDTH,
             DIFF_HEADS * 2 * DIFF_DQK, DIFF_HEADS * 2 * DIFF_DQK, DIFF_WIDTH]
IN_COLS = sum(IN_SPLITS)
IN_OFFSETS = [int(o) for o in np.cumsum(IN_SPLITS)[:-1]]

PEER_HEADS = 8
PEER_TOPK = 16
N_KEYS = 128
N_EXPERTS = N_KEYS * N_KEYS
PEER_DQ = 256
PEER_TOKEN_BLOCK = 128

RMS_EPS = 1e-6
NEG_INF = -1e30

kernel_name = "hybrid_retnet_diffattn_peer"


def rmsnorm(x, g=None):
    xf = x.astype(jnp.float32)
    y = xf * lax.rsqrt(jnp.mean(xf * xf, axis=-1, keepdims=True) + RMS_EPS)
    if g is not None:
        y = y * g.astype(jnp.float32)
    return y.astype(x.dtype)


def lambda_init(layer_idx):
    return 0.8 - 0.6 * float(np.exp(-0.3 * layer_idx))


def retention(q, k, v):
    B, T, H, dk = q.shape
    dv = v.shape[-1]
    C = RET_CHUNK
    N = T // C
    log_gamma = jnp.log1p(-jnp.exp2(-5.0 - jnp.arange(H, dtype=jnp.float32)))

    def chunks(a):
        return a.astype(jnp.float32).reshape(B, N, C, H, a.shape[-1]).transpose(1, 0, 3, 2, 4)

    qc, kc, vc = chunks(q), chunks(k) * (dk ** -0.5), chunks(v)
    idx = jnp.arange(C, dtype=jnp.float32)
    rel = idx[:, None] - idx[None, :]
    intra = jnp.where(rel >= 0, jnp.exp(log_gamma[:, None, None] * jnp.maximum(rel, 0.0)), 0.0)
    cross = jnp.exp(log_gamma[:, None] * (idx + 1.0))
    kdec = jnp.exp(log_gamma[:, None] * (C - 1.0 - idx))
    cdec = jnp.exp(log_gamma * C)

    def step(S, inp):
        qi, ki, vi = inp
        o = jnp.einsum('bhij,bhjv->bhiv', jnp.einsum('bhid,bhjd->bhij', qi, ki) * intra[None], vi)
        o = o + jnp.einsum('bhid,bhdv->bhiv', qi, S) * cross[None, :, :, None]
        S = S * cdec[None, :, None, None] + jnp.einsum('bhjd,bhjv->bhdv', ki * kdec[None, :, :, None], vi)
        return S, o

    S0 = jnp.zeros((B, H, dk, dv), jnp.float32)
    _, o = lax.scan(step, S0, (qc, kc, vc))
    return o.transpose(1, 0, 3, 2, 4).reshape(B, T, H, dv)


def alibi_slopes(n_heads):
    return jnp.exp2(-8.0 * (jnp.arange(n_heads, dtype=jnp.float32) + 1.0) / n_heads)


def diff_attention(q, k, v, lam):
    B, T, H, _, d = q.shape
    dv = v.shape[-1]
    C = Q_BLOCK
    N = T // C
    qb = (q.astype(jnp.float32) * (d ** -0.5)).reshape(B, N, C, H, 2, d).transpose(1, 0, 3, 4, 2, 5)
    kt = k.astype(jnp.float32).transpose(0, 2, 3, 1, 4)
    vt = v.astype(jnp.float32).transpose(0, 2, 1, 3)
    slopes = alibi_slopes(H)
    kpos = jnp.arange(T)

    def block(args):
        qi, start = args
        qpos = start + jnp.arange(C)
        dist = (qpos[:, None] - kpos[None, :]).astype(jnp.float32)
        bias = jnp.where(dist >= 0, -slopes[:, None, None] * dist, NEG_INF)
        s = jnp.einsum('bhpid,bhpjd->bhpij', qi, kt) + bias[None, :, None]
        p = jax.nn.softmax(s, axis=-1)
        a = p[:, :, 0] - lam * p[:, :, 1]
        return jnp.einsum('bhij,bhjv->bhiv', a, vt)

    o = lax.map(block, (qb, jnp.arange(N) * C))
    return o.transpose(1, 0, 3, 2, 4).reshape(B, T, H, dv)


def peer(xn, wq, subkeys, u, v):
    B, T, D = xn.shape
    xt = xn.reshape(-1, PEER_TOKEN_BLOCK, D)
    K = PEER_TOPK

    def block(xb):
        t = xb.shape[0]
        q = (xb @ wq).reshape(t, PEER_HEADS, 2, PEER_DQ // 2)
        s = jnp.einsum('thpd,hpkd->thpk', q, subkeys).astype(jnp.float32)
        s_top, i_top = lax.top_k(s, K)
        cand = (s_top[:, :, 0, :, None] + s_top[:, :, 1, None, :]).reshape(t, PEER_HEADS, K * K)
        c_top, c_idx = lax.top_k(cand, K)
        i1 = jnp.take_along_axis(i_top[:, :, 0], c_idx // K, axis=-1)
        i2 = jnp.take_along_axis(i_top[:, :, 1], c_idx % K, axis=-1)
        e = i1 * N_KEYS + i2
        g = jax.nn.softmax(c_top, axis=-1)
        ue = jnp.take(u, e, axis=0)
        ve = jnp.take(v, e, axis=0)
        act = jax.nn.gelu(jnp.einsum('thkd,td->thk', ue, xb).astype(jnp.float32), approximate=False)
        return jnp.einsum('thk,thkd->td', (g * act).astype(xb.dtype), ve)

    return lax.map(block, xt).reshape(B, T, D)


def setup_inputs(seed: int = 0) -> dict:
    key = jax.random.key(seed)
    ks = jax.random.split(key, 13)
    f = jnp.float32
    x = jax.random.normal(ks[0], (BATCH, SEQ, D_MODEL), f)
    norm_mix_g = 1.0 + 0.02 * jax.random.normal(ks[1], (DEPTH, D_MODEL), f)
    w_in = jax.random.normal(ks[2], (DEPTH, D_MODEL, IN_COLS), f) * D_MODEL ** -0.5
    diff_lambda = 0.1 * jax.random.normal(ks[3], (DEPTH, 4, DIFF_DQK), f)
    diff_subln_g = 1.0 + 0.02 * jax.random.normal(ks[4], (DEPTH, DIFF_DV), f)
    w_out = jax.random.normal(ks[5], (DEPTH, MIX_WIDTH, D_MODEL), f) * MIX_WIDTH ** -0.5
    norm_ffn_g = 1.0 + 0.02 * jax.random.normal(ks[6], (DEPTH, D_MODEL), f)
    peer_wq = jax.random.normal(ks[7], (DEPTH, D_MODEL, PEER_HEADS * PEER_DQ), f) * D_MODEL ** -0.5
    peer_subkeys = jax.random.normal(ks[8], (DEPTH, PEER_HEADS, 2, N_KEYS, PEER_DQ // 2), f) * (PEER_DQ // 2) ** -0.5
    peer_u = jax.random.normal(ks[9], (DEPTH, N_EXPERTS, D_MODEL), f) * D_MODEL ** -0.5
    peer_v = jax.random.normal(ks[10], (DEPTH, N_EXPERTS, D_MODEL), f) * PEER_TOPK ** -0.5
    final_norm_g = 1.0 + 0.02 * jax.random.normal(ks[11], (D_MODEL,), f)
    return {"x": x, "norm_mix_g": norm_mix_g, "w_in": w_in, "diff_lambda": diff_lambda,
            "diff_subln_g": diff_subln_g, "w_out": w_out, "norm_ffn_g": norm_ffn_g,
            "peer_wq": peer_wq, "peer_subkeys": peer_subkeys, "peer_u": peer_u,
            "peer_v": peer_v, "final_norm_g": final_norm_g}


def reference(x, norm_mix_g, w_in, diff_lambda, diff_subln_g, w_out, norm_ffn_g,
              peer_wq, peer_subkeys, peer_u, peer_v, final_norm_g):
    B, T, D = x.shape
    for l in range(DEPTH):
        h = rmsnorm(x, norm_mix_g[l])
        proj = h @ w_in[l]
        rq, rk, rv, rg, dq, dk, dv = jnp.split(proj, IN_OFFSETS, axis=-1)
        ry = retention(rq.reshape(B, T, RET_HEADS, RET_DK), rk.reshape(B, T, RET_HEADS, RET_DK),
                       rv.reshape(B, T, RET_HEADS, RET_DV))
        ry = rmsnorm(ry).reshape(B, T, RET_WIDTH)
        ret_out = (jax.nn.silu(rg.astype(jnp.float32)) * ry).astype(x.dtype)
        li = lambda_init(l)
        lp = diff_lambda[l].astype(jnp.float32)
        lam = jnp.exp(jnp.sum(lp[0] * lp[1])) - jnp.exp(jnp.sum(lp[2] * lp[3])) + li
        dy = diff_attention(dq.reshape(B, T, DIFF_HEADS, 2, DIFF_DQK), dk.reshape(B, T, DIFF_HEADS, 2, DIFF_DQK),
                            dv.reshape(B, T, DIFF_HEADS, DIFF_DV), lam)
        diff_out = (rmsnorm(dy, diff_subln_g[l]) * (1.0 - li)).reshape(B, T, DIFF_WIDTH).astype(x.dtype)
        x = x + jnp.concatenate([ret_out, diff_out], axis=-1) @ w_out[l]
        x = x + peer(rmsnorm(x, norm_ffn_g[l]), peer_wq[l], peer_subkeys[l], peer_u[l], peer_v[l])
    return rmsnorm(x, final_norm_g)
```

```python
import os
import numpy as np
import concourse.bass as bass
import concourse.mybir as mybir
from concourse.bass_utils import run_bass_kernel_spmd

F32 = mybir.dt.float32
BF16 = mybir.dt.bfloat16
I32 = mybir.dt.int32
U32 = mybir.dt.uint32
ALU = mybir.AluOpType
AF = mybir.ActivationFunctionType
AX = mybir.AxisListType

ENGS = ("pe", "act", "dve", "pool", "sp")
NCORES = 8
BPC = 2
T = 2048
D = 1024
NT = T // 128
RMS_EPS = 1e-6
LAMBDA_INIT = 0.8 - 0.6 * float(np.exp(-0.3 * 0))


class Res:
    __slots__ = ("name", "w", "r", "dsem", "dcnt", "excl")

    def __init__(self, name):
        self.name = name
        self.excl = False
        self.w = None
        self.r = []
        self.dsem = None
        self.dcnt = 0


class Prog:
    def __init__(self, nc, same_engine_sync=("act", "dve", "pool")):
        self.nc = nc
        self.ops = {e: [] for e in ENGS}
        self.waited = {e: {} for e in ENGS}
        self.same = set(same_engine_sync)
        self.ctx = []
        self.esem = {}
        self.pending = {e: [] for e in ENGS}
        self.all_res = []
        self.capture = None

    def res(self, name):
        r = Res(name)
        self.all_res.append(r)
        return r

    def barrier(self):
        tks = []
        for r in self.all_res:
            if r.w is not None:
                tks.append(r.w)
            tks.extend(r.r)
        for e in ENGS:
            self.pending[e] = list(tks)

    def sb(self, name, shape, dt):
        g = self.nc.sbuf_tensor(name, list(shape), dt)
        t = g.__enter__()
        self.ctx.append(g)
        return t

    def ps(self, name, shape, dt):
        g = self.nc.psum_tensor(name, list(shape), dt)
        t = g.__enter__()
        self.ctx.append(g)
        return t

    def sem(self, name):
        g = self.nc.semaphore(name)
        s = g.__enter__()
        self.ctx.append(g)
        return s

    def _need(self, eng, tk, waits):
        if tk is None:
            return
        if tk[0] == "e":
            _, e2, seq = tk
            if e2 == eng and eng not in self.same:
                return
            key = ("e", e2)
            if self.waited[eng].get(key, 0) >= seq:
                return
            self.waited[eng][key] = seq
            self.ops[e2][seq - 1]["signal"] = True
            waits.append(tk)
        else:
            _, res, val = tk
            key = ("d", id(res))
            if self.waited[eng].get(key, 0) >= val:
                return
            self.waited[eng][key] = val
            waits.append(tk)

    def _deps(self, eng, reads, writes):
        waits = []
        if self.pending[eng]:
            for tk in self.pending[eng]:
                self._need(eng, tk, waits)
            self.pending[eng] = []
        for r in reads:
            self._need(eng, r.w, waits)
        for w in writes:
            self._need(eng, w.w, waits)
            for t in w.r:
                self._need(eng, t, waits)
        return waits

    def replay(self, items):
        for kind, eng, fn, reads, writes, chan in items:
            if kind == "op":
                self.op(eng, fn, reads, writes)
            else:
                self.dma(eng, fn, reads, writes, chan)

    def op(self, eng, fn, reads=(), writes=(), cost=None):
        if self.capture is not None:
            if cost is None:
                cost = 0.3 if eng == "dve" else 0.0
            self.capture.append(("op", eng, fn, list(reads), list(writes), cost))
            return None
        xr = [r for r in reads if r.excl]
        if xr:
            reads = [r for r in reads if not r.excl]
            writes = list(writes) + [r for r in xr if r not in writes]
        waits = self._deps(eng, reads, writes)
        self.ops[eng].append(dict(fn=fn, waits=waits, signal=False, dma=None))
        tk = ("e", eng, len(self.ops[eng]))
        for r in reads:
            r.r.append(tk)
        for w in writes:
            w.w = tk
            w.r = []
        return tk

    def dma(self, eng, fn, reads=(), writes=(), chan=None):
        if self.capture is not None:
            self.capture.append(("dma", eng, fn, list(reads), list(writes), chan))
            return None
        waits = self._deps(eng, reads, writes)
        if chan is None:
            chan = writes[0] if writes else reads[0]
        if chan.dsem is None:
            chan.dsem = self.sem("d_" + chan.name)
        chan.dcnt += 16
        self.ops[eng].append(dict(fn=fn, waits=waits, signal=False, dma=(chan, chan.dcnt)))
        tk = ("d", chan, chan.dcnt)
        for r in reads:
            r.r.append(tk)
        for w in writes:
            w.w = tk
            w.r = []
        return tk

    def emit(self, final_waits=()):
        nc = self.nc
        for e in ENGS:
            self.esem[e] = self.sem("e_" + e)
        fw = []
        for r in final_waits:
            self._need("sp", r.w, fw)
            for t in r.r:
                self._need("sp", t, fw)
        self.ops["sp"].append(dict(fn=None, waits=fw, signal=False, dma=None))
        val = {}
        for e in ENGS:
            c = 0
            v = []
            for o in self.ops[e]:
                if o["signal"]:
                    c += 1
                v.append(c)
            val[e] = v

        def run(e, eo):
            for o in self.ops[e]:
                for tk in o["waits"]:
                    if tk[0] == "e":
                        eo.wait_ge(self.esem[tk[1]], val[tk[1]][tk[2] - 1])
                    else:
                        eo.wait_ge(tk[1].dsem, tk[2])
                if o["fn"] is None:
                    continue
                ins = o["fn"](eo)
                if o["dma"] is not None:
                    ins.then_inc(o["dma"][0].dsem, 16)
                elif o["signal"]:
                    ins.then_inc(self.esem[e], 1)

        with nc.Block() as block:
            @block.sync
            def _(eo):
                run("sp", eo)

            @block.tensor
            def _(eo):
                run("pe", eo)

            @block.scalar
            def _(eo):
                run("act", eo)

            @block.vector
            def _(eo):
                run("dve", eo)

            @block.gpsimd
            def _(eo):
                run("pool", eo)

    def close(self):
        for g in reversed(self.ctx):
            g.__exit__(None, None, None)
        self.ctx = []


def host_consts():
    f = np.float32
    H = 4
    C = 128
    idx = np.arange(C, dtype=f)
    log_gamma = np.log1p(-np.exp2(-5.0 - np.arange(H, dtype=f))).astype(f)
    rel = idx[None, :] - idx[:, None]
    DT = np.where(rel >= 0, np.exp(log_gamma[:, None, None] * np.maximum(rel, 0.0)[None]), 0.0)
    DT = (DT * (128.0 ** -0.5)).astype(f)
    cross = np.exp(log_gamma[:, None] * (idx + 1.0)).astype(f)
    kdec = (np.exp(log_gamma[:, None] * (C - 1.0 - idx)) * (128.0 ** -0.5)).astype(f)
    cdec = np.exp(log_gamma * C).astype(f)
    slopes = np.exp2(-8.0 * (np.arange(H, dtype=f) + 1.0) / H).astype(f)
    jp = idx
    bias = np.zeros((C, H, 16), f)
    for h in range(H):
        for dlt in range(16):
            bias[:, h, dlt] = slopes[h] * (jp - 127.0) - slopes[h] * 128.0 * dlt
    c = {}
    c["c_ident"] = np.eye(128, dtype=f)
    c["c_DT"] = np.ascontiguousarray(DT.transpose(1, 0, 2))
    c["c_cross"] = np.ascontiguousarray(np.broadcast_to(cross[None], (128, H, C))).astype(f)
    c["c_kdec"] = np.ascontiguousarray(kdec.T)
    c["c_bias"] = bias
    c["c_mask"] = (idx[:, None] <= idx[None, :]).astype(f)
    c["c_iota16"] = np.ascontiguousarray(np.broadcast_to(np.arange(16, dtype=f)[None], (128, 16)))
    c["c_thr"] = np.ascontiguousarray(np.broadcast_to((16.0 * (np.arange(16, dtype=f) + 1.0))[None], (128, 16)))
    return c, [float(x) for x in cdec]


class _Stop(Exception):
    pass


def build_program(debug=False, bpc=BPC, peer=True, stop=None):
    consts, cdec = host_consts()
    nc = bass.Bass("TRN2", target_bir_lowering=False)
    dr = {}

    def din(name, shape, dt=F32):
        dr[name] = nc.dram_tensor(name, list(shape), dt, kind="ExternalInput").ap()
        return dr[name]

    x_d = din("x", [bpc, T, D])
    w_in_d = din("w_in", [D, 3584])
    w_out_d = din("w_out", [D, D])
    wq_d = din("wq", [D, 2048])
    skT_d = din("skT", [128, 16, 128])
    u_d = din("u", [16384, D])
    v_d = din("v", [16384, D])
    gmix_d = din("gmix_col", [128, 8])
    gffn_d = din("gffn_b", [128, D])
    gfin_d = din("gfin_b", [128, D])
    gsub_d = din("gsub_b", [128, 128])
    dl_d = din("dl_b", [128, 4, 64])
    for k, a in consts.items():
        din(k, a.shape)
    uv_d = nc.dram_tensor("uv_scr", [16384, 2 * D], BF16, kind="Internal").ap()
    out_d = nc.dram_tensor("out", [bpc, T, D], F32, kind="ExternalOutput").ap()
    if debug:
        dbg_d = nc.dram_tensor("dbg", [bpc, T, D], F32, kind="ExternalOutput").ap()

    P = Prog(nc)
    R = {}

    def sbt(name, shape, dt, nres=1):
        t = P.sb(name, shape, dt)
        R[name] = P.res(name)
        return t

    UW = 22944
    U = P.sb("U", [128, UW], F32)
    uoff = {"o": 0}

    def carve(name, shape, dt):
        n = int(np.prod(shape[1:]))
        esz = 2 if dt == BF16 else 4
        words = (n * esz + 3) // 4
        words += words % 2
        assert uoff["o"] + words <= UW, (name, uoff["o"], words)
        ap = U[:, uoff["o"]:uoff["o"] + words]
        uoff["o"] += words
        if dt != F32:
            ap = ap.bitcast(dt)
        ap = ap[:, 0:n]
        if len(shape) == 3:
            ap = ap.rearrange("p (a b) -> p a b", b=shape[2])
        R[name] = P.res(name)
        return ap

    ident_f = sbt("ident_f", [128, 128], F32)
    ident_b = sbt("ident_b", [128, 128], BF16)
    DT_sb = sbt("DT", [128, 4, 128], F32)
    cross_sb = sbt("cross", [128, 4, 128], F32)
    kdec_sb = sbt("kdec", [128, 4], F32)
    bias_sb = sbt("bias", [128, 4, 16], F32)
    mask_sb = sbt("mask", [128, 128], F32)
    iota16 = sbt("iota16", [128, 16], F32)
    thr = sbt("thr", [128, 16], F32)
    gmix = sbt("gmix", [128, 8], F32)
    gffn = sbt("gffn", [128, D], F32)
    gfin = sbt("gfin", [128, D], F32)
    gsub = sbt("gsub", [128, 128], F32)
    dl = sbt("dl", [128, 4, 64], F32)
    lamt = sbt("lamt", [128, 8], F32)
    w_out_b = sbt("w_out_b", [128, 8, D], BF16)
    wq_b = sbt("wq_b", [128, 8, 2048], BF16)
    skT_b = sbt("skT_b", [128, 16, 128], BF16)

    def load(dst, src, name, eng="sp"):
        P.dma(eng, lambda e: e.dma_start(out=dst, in_=src), writes=[R[name]])

    load(ident_f[:], dr["c_ident"], "ident_f")
    load(DT_sb[:], dr["c_DT"], "DT")
    load(cross_sb[:], dr["c_cross"], "cross")
    load(kdec_sb[:], dr["c_kdec"], "kdec")
    load(bias_sb[:], dr["c_bias"], "bias")
    load(mask_sb[:], dr["c_mask"], "mask")
    load(iota16[:], dr["c_iota16"], "iota16")
    load(thr[:], dr["c_thr"], "thr")
    load(gmix[:], gmix_d, "gmix")
    load(gffn[:], gffn_d, "gffn")
    load(gfin[:], gfin_d, "gfin")
    load(gsub[:], gsub_d, "gsub")
    load(dl[:], dl_d, "dl")
    load(w_out_b[:], w_out_d.rearrange("(c p) n -> p c n", p=128), "w_out_b", eng="pool")
    for c in range(8):
        P.dma("pool", lambda e, c=c: e.dma_start(out=wq_b[:, c, :], in_=wq_d[c * 128:(c + 1) * 128, :]),
              writes=[R["wq_b"]])
    load(skT_b[:], skT_d, "skT_b", eng="pool")
    P.op("dve", lambda e: e.tensor_copy(out=ident_b[:], in_=ident_f[:]), reads=[R["ident_f"]], writes=[R["ident_b"]])

    junk64 = sbt("junk64", [128, 64], F32)
    P.op("dve", lambda e: e.memset(lamt[:], 0.0), writes=[R["lamt"]])
    P.op("dve", lambda e: e.scalar_tensor_tensor(out=junk64[:], in0=dl[:, 0, :], scalar=1.0, in1=dl[:, 1, :],
                                                 op0=ALU.mult, op1=ALU.mult, accum_out=lamt[:, 0:1]),
         reads=[R["dl"]], writes=[R["junk64"], R["lamt"]])
    P.op("dve", lambda e: e.scalar_tensor_tensor(out=junk64[:], in0=dl[:, 2, :], scalar=1.0, in1=dl[:, 3, :],
                                                 op0=ALU.mult, op1=ALU.mult, accum_out=lamt[:, 1:2]),
         reads=[R["dl"]], writes=[R["junk64"], R["lamt"]])
    P.op("act", lambda e: e.activation(out=lamt[:, 2:4], in_=lamt[:, 0:2], func=AF.Exp),
         reads=[R["lamt"]], writes=[R["lamt"]])
    P.op("dve", lambda e: e.tensor_tensor(out=lamt[:, 4:5], in0=lamt[:, 3:4], in1=lamt[:, 2:3], op=ALU.subtract),
         reads=[R["lamt"]], writes=[R["lamt"]])
    P.op("dve", lambda e: e.tensor_scalar(out=lamt[:, 5:6], in0=lamt[:, 4:5], scalar1=-LAMBDA_INIT, scalar2=None,
                                          op0=ALU.add), reads=[R["lamt"]], writes=[R["lamt"]])
    neglam = lamt[:, 5:6]

    cat = sbt("cat", [128, NT, D], BF16)
    xt = [sbt(f"xt{i}", [128, D], F32) for i in range(2)]
    xb = sbt("xb", [128, D], BF16)
    junk = sbt("junk", [128, D], F32)
    st = sbt("st", [128, 16], F32)
    S_f = sbt("S_f", [128, 128], F32)
    S_b = sbt("S_b", [128, 128], BF16)
    stT = [sbt(f"stT{i}", [128, 256], BF16) for i in range(3)]
    o_sb = sbt("o_sb", [128, 128], F32)
    t1 = sbt("t1", [128, 128], F32)
    mask_b = sbt("mask_b", [128, 128], BF16)
    uoff["o"] = 0
    hT = carve("hT", [128, 8, T], BF16)
    wtok = carve("wtok", [128, 8, 512], BF16)
    wfeat = carve("wfeat", [128, 8, 512], BF16)
    qTr = carve("qTr", [128, T], BF16)
    qTrc = carve("qTrc", [128, T], BF16)
    kTr = carve("kTr", [128, T], BF16)
    qTd = carve("qTd", [128, T], BF16)
    kTd = carve("kTd", [128, T], BF16)
    kd = carve("kd", [128, NT, 128], BF16)
    rv = carve("rv", [128, NT, 128], BF16)
    sg = carve("sg", [128, NT, 128], BF16)
    dv = carve("dv", [128, NT, 132], BF16)
    uoff["o"] = 0
    catT = carve("catT", [128, 8, 128], BF16)
    x1 = carve("x1", [128, D], F32)
    xn = None
    xnT = carve("xnT", [128, 8, 128], BF16)
    qT_sb = carve("qT_sb", [128, 16, 128], BF16)
    sc = carve("sc", [128, 16, 128], F32)
    top = carve("top", [128, 16, 16], F32)
    itop = carve("itop", [128, 16, 16], U32)
    itopf = carve("itopf", [128, 16, 16], F32)
    cand = carve("cand", [128, 8, 256], F32)
    ctop = carve("ctop", [128, 8, 16], F32)
    cidx = carve("cidx", [128, 8, 16], U32)
    cidxf = carve("cidxf", [128, 128], F32)
    r12f = carve("r12f", [128, 2, 128], F32)
    eq = cand
    R["eq"] = R["cand"]
    i12f = carve("i12f", [128, 2, 128], F32)
    ef = carve("ef", [128, 128], F32)
    eidx = carve("eidx", [128, 128], I32)
    gate = carve("gate", [128, 128], F32)
    gz = carve("gz", [128, 16], F32)
    actv = carve("actv", [128, 128], F32)
    coef = carve("coef", [128, 128], F32)
    NG = 9
    gbuf = [carve(f"gbuf{i}", [128, 2 * D], BF16) for i in range(NG)]
    dg = [carve(f"dg{i}", [128, 128], BF16) for i in range(2)]
    x2 = carve("x2", [128, D], F32)
    ob = x2
    R["ob"] = R["x2"]
    jkb = [carve("jkb0", [128, D], BF16)] * 2
    R["jkb1"] = R["jkb0"]
    x1s = [x1, carve("x1_b", [128, D], F32)]
    xns = [None, None]
    eidxs = [eidx, carve("eidx_b", [128, 128], I32)]
    gates = [gate, carve("gate_b", [128, 128], F32)]
    xnbs = [carve("xnb_0", [128, D], BF16), carve("xnb_1", [128, D], BF16)]
    for nm in ("x1", "eidx", "gate"):
        R[nm + "_0"] = R[nm]
        R[nm + "_1"] = R[nm + "_b"]
    print("P34 union words used", uoff["o"], "of", UW)
    cf = carve("cf", [128, 128], F32)
    cf2 = carve("cf2", [128, 128], F32)
    for i in range(8):
        R[f"actv{i}"] = P.res(f"actv{i}")
        R[f"cf{i}"] = P.res(f"cf{i}")

    pA = P.ps("pA", [128, 512], F32); R["pA"] = P.res("pA"); R["pA"].excl = True
    pB = P.ps("pB", [128, 512], F32); R["pB"] = P.res("pB"); R["pB"].excl = True
    pCD = P.ps("pCD", [128, 1024], F32)
    pC = pCD[:, 0:512]; R["pC"] = P.res("pC"); R["pC"].excl = True
    pD = pCD[:, 512:1024]; R["pD"] = P.res("pD"); R["pD"].excl = True
    pE = P.ps("pE", [128, 512], F32); R["pE"] = P.res("pE"); R["pE"].excl = True
    pF = P.ps("pF", [128, 512], F32); R["pF"] = P.res("pF"); R["pF"].excl = True
    pT01 = P.ps("pT01", [128, 2, 1024], BF16)
    pT0 = pT01[:, 0, :].rearrange("p (c t) -> p c t", t=128); R["pT0"] = P.res("pT0"); R["pT0"].excl = True
    pT1 = pT01[:, 1, :].rearrange("p (c t) -> p c t", t=128); R["pT1"] = P.res("pT1"); R["pT1"].excl = True
    pT01f = pT01[:].rearrange("p a b -> p (a b)").bitcast(F32)

    P.op("dve", lambda e: e.tensor_copy(out=mask_b[:], in_=mask_sb[:]), reads=[R["mask"]], writes=[R["mask_b"]])

    uoff["o"] = 0
    NSTG = 4
    stg = [carve(f"stg{i}", [128, 8, D], BF16) for i in range(NSTG)]
    for i in range(NSTG):
        R[f"scr{i}"] = P.res(f"scr{i}")
    k = 0
    for src, dst in ((u_d, uv_d[:, 0:D]), (v_d, uv_d[:, D:2 * D])):
        srcv = src.rearrange("(p k) d -> p k d", p=128)
        dstv = dst.rearrange("(p k) d -> p k d", p=128)
        for c in range(16):
            sb_i = k % NSTG
            k += 1
            P.dma("pool", lambda e, srcv=srcv, c=c, sb_i=sb_i: e.dma_start(out=stg[sb_i][:], in_=srcv[:, c * 8:(c + 1) * 8, :]),
                  writes=[R[f"stg{sb_i}"]])
            P.dma("sp", lambda e, dstv=dstv, c=c, sb_i=sb_i: e.dma_start(out=dstv[:, c * 8:(c + 1) * 8, :], in_=stg[sb_i][:]),
                  reads=[R[f"stg{sb_i}"]], writes=[R[f"scr{sb_i}"]])

    def rms_stats(src_ap, src_res, ncols, col):
        jk = junk[:, 0:ncols]
        P.op("dve", lambda e: e.memset(st[:, col:col + 1], 0.0), writes=[R["st"]])
        if ncols == D:
            P.op("act", lambda e: e.activation(out=jk, in_=src_ap, func=AF.Square, accum_out=st[:, col:col + 1]),
                 reads=list(src_res), writes=[R["junk"], R["st"]])
        else:
            P.op("dve", lambda e: e.scalar_tensor_tensor(out=jk, in0=src_ap, scalar=1.0, in1=src_ap,
                                                         op0=ALU.mult, op1=ALU.mult, accum_out=st[:, col:col + 1]),
                 reads=list(src_res), writes=[R["junk"], R["st"]], cost=0.2 + ncols / 900.0)
        P.op("dve", lambda e: e.tensor_scalar(out=st[:, col:col + 1], in0=st[:, col:col + 1],
                                              scalar1=1.0 / ncols, scalar2=RMS_EPS, op0=ALU.mult, op1=ALU.add),
             reads=[R["st"]], writes=[R["st"]])
        P.op("act", lambda e: e.activation(out=st[:, col:col + 1], in_=st[:, col:col + 1], func=AF.Sqrt),
             reads=[R["st"]], writes=[R["st"]])
        P.op("dve", lambda e: e.reciprocal(out=st[:, col:col + 1], in_=st[:, col:col + 1]),
             reads=[R["st"]], writes=[R["st"]])

    def transposes8(src_t, src_res, ptile, pname):
        for c in range(8):
            P.op("pe", lambda e, c=c: e.transpose(out=ptile[:, c, :], in_=src_t[:, c * 128:(c + 1) * 128],
                                                  identity=ident_b[:]),
                 reads=[src_res, R["ident_b"]], writes=[R[pname]])

    def _main_body(chk):
        for b in range(bpc):
            chk(0)
            P.barrier()
            for i in range(NT):
                xti = xt[i % 2]
                xr = f"xt{i % 2}"
                P.dma("sp", lambda e, xti=xti, b=b, i=i: e.dma_start(out=xti[:], in_=x_d[b, i * 128:(i + 1) * 128, :]),
                      writes=[R[xr]])
                rms_stats(xti[:], [R[xr]], D, 0)
                P.op("act", lambda e, xti=xti: e.activation(out=xb[:], in_=xti[:], func=AF.Copy, scale=st[:, 0:1]),
                     reads=[R[xr], R["st"]], writes=[R["xb"]])
                pt, pn = (pT0, "pT0") if i % 2 == 0 else (pT1, "pT1")
                transposes8(xb, R["xb"], pt, pn)
                P.op("dve", lambda e, pt=pt, i=i: e.tensor_tensor(
                    out=hT[:, :, i * 128:(i + 1) * 128], in0=pt[:],
                    in1=gmix[:].unsqueeze(2).to_broadcast([128, 8, 128]), op=ALU.mult),
                    reads=[R[pn], R["gmix"]], writes=[R["hT"]])

            chk(1)
            if not os.environ.get('KNOMEMSET'):
                P.op("pool", lambda e: e.memset(dv[:], 1.0), writes=[R["dv"]])
            for h in range(4):
                tok_cols = [512 + h * 128, 1024 + h * 128, 1536 + h * 128, 3072 + h * 128]
                feat_cols = [h * 128, 512 + h * 128, 2048 + h * 128, 2560 + h * 128]
                for k, c0 in enumerate(tok_cols):
                    P.dma("pool", lambda e, k=k, c0=c0: e.dma_start(
                        out=wtok[:, :, k * 128:(k + 1) * 128],
                        in_=w_in_d[:, c0:c0 + 128].rearrange("(c p) n -> p c n", p=128)), writes=[R["wtok"]])
                for k, c0 in enumerate(feat_cols):
                    P.dma("pool", lambda e, k=k, c0=c0: e.dma_start(
                        out=wfeat[:, :, k * 128:(k + 1) * 128],
                        in_=w_in_d[:, c0:c0 + 128].rearrange("(c p) n -> p c n", p=128)), writes=[R["wfeat"]])
                fdst = [(qTr, "qTr", 1.0), (kTr, "kTr", 1.0), (qTd, "qTd", 0.125), (kTd, "kTd", 1.0)]
                cnt = 0
                for k, (dst, dn, scl) in enumerate(fdst):
                    for tb in range(4):
                        pp, pn = (pA, "pA") if cnt % 2 == 0 else (pB, "pB")
                        cnt += 1
                        for c in range(8):
                            P.op("pe", lambda e, pp=pp, k=k, c=c, tb=tb: e.matmul(
                                out=pp[:], lhsT=wfeat[:, c, k * 128:(k + 1) * 128], rhs=hT[:, c, tb * 512:(tb + 1) * 512],
                                start=(c == 0), stop=(c == 7)),
                                reads=[R["wfeat"], R["hT"]], writes=[R[pn]])
                        P.op("act", lambda e, pp=pp, dst=dst, tb=tb, scl=scl: e.activation(
                            out=dst[:, tb * 512:(tb + 1) * 512], in_=pp[:], func=AF.Copy, scale=scl),
                            reads=[R[pn]], writes=[R[dn]])
                P.op("dve", lambda e, h=h: e.tensor_tensor(
                    out=qTrc[:].rearrange("p (n i) -> p n i", i=128), in0=qTr[:].rearrange("p (n i) -> p n i", i=128),
                    in1=cross_sb[:, h:h + 1, :].to_broadcast([128, NT, 128]), op=ALU.mult),
                    reads=[R["qTr"], R["cross"]], writes=[R["qTrc"]])
                chk(2)
                for ti in range(NT):
                    pp, pn = (pA, "pA") if ti % 2 == 0 else (pB, "pB")
                    for c in range(8):
                        P.op("pe", lambda e, pp=pp, c=c, ti=ti: e.matmul(
                            out=pp[:], lhsT=hT[:, c, ti * 128:(ti + 1) * 128], rhs=wtok[:, c, :],
                            start=(c == 0), stop=(c == 7)),
                            reads=[R["wtok"], R["hT"]], writes=[R[pn]])
                    KT = int(os.environ.get('KTOK', '15'))
                    if KT & 1:
                        P.op("dve", lambda e, pp=pp, ti=ti, h=h: e.tensor_scalar(
                            out=kd[:, ti, :], in0=pp[:, 0:128], scalar1=kdec_sb[:, h:h + 1], scalar2=None, op0=ALU.mult),
                            reads=[R[pn], R["kdec"]], writes=[R["kd"]])
                    if KT & 2:
                        P.op("act", lambda e, pp=pp, ti=ti: e.activation(out=rv[:, ti, :], in_=pp[:, 128:256], func=AF.Copy, scale=1.0),
                             reads=[R[pn]], writes=[R["rv"]])
                    if KT & 4:
                        P.op("act", lambda e, pp=pp, ti=ti: e.activation(out=sg[:, ti, :], in_=pp[:, 256:384], func=(AF.Copy if os.environ.get('KNOSILU') else AF.Silu)),
                             reads=[R[pn]], writes=[R["sg"]])
                    if KT & 8:
                        P.op("dve", lambda e, pp=pp, ti=ti: e.tensor_copy(out=dv[:, ti, 0:128], in_=pp[:, 384:512]),
                             reads=[R[pn]], writes=[R["dv"]])
                chk(3)
                for n in range(NT):
                    sl = slice(n * 128, (n + 1) * 128)
                    sb_i = n % 2
                    P.op("pe", lambda e, sl=sl: e.matmul(out=pC[:, 0:128], lhsT=kTr[:, sl], rhs=qTr[:, sl],
                                                          start=True, stop=True),
                         reads=[R["kTr"], R["qTr"]], writes=[R["pC"]])
                    P.op("dve", lambda e, sb_i=sb_i, h=h: e.tensor_tensor(out=stT[sb_i][:, 0:128], in0=pC[:, 0:128],
                                                                          in1=DT_sb[:, h, :], op=ALU.mult),
                         reads=[R["pC"], R["DT"]], writes=[R[f"stT{sb_i}"]])
                    P.op("pe", lambda e, sb_i=sb_i, n=n: e.matmul(out=pD[:, 0:128], lhsT=stT[sb_i][:, 0:128], rhs=rv[:, n, :],
                                                                  start=True, stop=(n == 0)),
                         reads=[R[f"stT{sb_i}"], R["rv"]], writes=[R["pD"]])
                    if n > 0:
                        P.op("pe", lambda e, sl=sl: e.matmul(out=pD[:, 0:128], lhsT=qTrc[:, sl], rhs=S_b[:],
                                                              start=False, stop=True),
                             reads=[R["qTrc"], R["S_b"]], writes=[R["pD"]])
                    P.op("act", lambda e: e.activation(out=o_sb[:], in_=pD[:, 0:128], func=AF.Copy),
                         reads=[R["pD"]], writes=[R["o_sb"]])
                    rms_stats(o_sb[:], [R["o_sb"]], 128, 1)
                    P.op("dve", lambda e, n=n, h=h: e.scalar_tensor_tensor(
                        out=cat[:, n, h * 128:(h + 1) * 128], in0=o_sb[:], scalar=st[:, 1:2], in1=sg[:, n, :],
                        op0=ALU.mult, op1=ALU.mult),
                        reads=[R["o_sb"], R["st"], R["sg"]], writes=[R["cat"]])
                    if n < NT - 1:
                        P.op("pe", lambda e, n=n: e.matmul(out=pA[:, 0:128], lhsT=kd[:, n, :], rhs=rv[:, n, :],
                                                            start=True, stop=True),
                             reads=[R["kd"], R["rv"]], writes=[R["pA"]])
                        if n == 0:
                            P.op("dve", lambda e: e.tensor_copy(out=S_f[:], in_=pA[:, 0:128]),
                                 reads=[R["pA"]], writes=[R["S_f"]])
                        else:
                            P.op("dve", lambda e, h=h: e.scalar_tensor_tensor(
                                out=S_f[:], in0=S_f[:], scalar=cdec[h], in1=pA[:, 0:128], op0=ALU.mult, op1=ALU.add),
                                reads=[R["pA"], R["S_f"]], writes=[R["S_f"]])
                        P.op("act", lambda e: e.activation(out=S_b[:], in_=S_f[:], func=AF.Copy),
                             reads=[R["S_f"]], writes=[R["S_b"]])
                chk(4)
                def combine(I, po0, pn0, po1, pn1, h=h):
                    P.op("dve", lambda e: e.reciprocal(out=st[:, 4:5], in_=po0[:, 128:129]), reads=[R[pn0]], writes=[R["st"]])
                    P.op("dve", lambda e: e.reciprocal(out=st[:, 5:6], in_=po1[:, 128:129]), reads=[R[pn1]], writes=[R["st"]])
                    P.op("dve", lambda e: e.tensor_tensor(out=st[:, 5:6], in0=st[:, 5:6], in1=neglam, op=ALU.mult),
                         reads=[R["st"], R["lamt"]], writes=[R["st"]])
                    P.op("dve", lambda e: e.tensor_scalar(out=t1[:], in0=po1[:, 0:128], scalar1=st[:, 5:6], scalar2=None,
                                                          op0=ALU.mult), reads=[R[pn1], R["st"]], writes=[R["t1"]])
                    P.op("dve", lambda e: e.scalar_tensor_tensor(out=o_sb[:], in0=po0[:, 0:128], scalar=st[:, 4:5], in1=t1[:],
                                                                 op0=ALU.mult, op1=ALU.add),
                         reads=[R[pn0], R["st"], R["t1"]], writes=[R["o_sb"]])
                    rms_stats(o_sb[:], [R["o_sb"]], 128, 6)
                    P.op("dve", lambda e: e.tensor_scalar(out=st[:, 6:7], in0=st[:, 6:7], scalar1=1.0 - LAMBDA_INIT,
                                                          scalar2=None, op0=ALU.mult), reads=[R["st"]], writes=[R["st"]])
                    P.op("dve", lambda e, I=I, h=h: e.scalar_tensor_tensor(
                        out=cat[:, I, 512 + h * 128:512 + (h + 1) * 128], in0=o_sb[:], scalar=st[:, 6:7], in1=gsub[:],
                        op0=ALU.mult, op1=ALU.mult),
                        reads=[R["o_sb"], R["st"], R["gsub"]], writes=[R["cat"]])


                items = [(I, J) for I in range(NT) for J in range(I + 1)]

                def acc(I):
                    return ((pE, "pE", pF, "pF") if I % 2 == 0 else (pA, "pA", pB, "pB"))

                def emit_S(k):
                    I, J = items[k]
                    isl = slice(I * 128, (I + 1) * 128)
                    jsl = slice(J * 128, (J + 1) * 128)
                    if k % 2 == 0:
                        pair, pns = pCD, ("pC", "pD")
                    else:
                        pair, pns = pT01f, ("pT0", "pT1")
                    sb_i = k % 3
                    for m in range(2):
                        msl = slice(m * 64, (m + 1) * 64)
                        P.op("pe", lambda e, pair=pair, msl=msl, jsl=jsl, isl=isl, m=m: e.matmul(
                            out=pair[:, m * 512:m * 512 + 128], lhsT=kTd[msl, jsl], rhs=qTd[msl, isl], start=True, stop=True),
                            reads=[R["kTd"], R["qTd"]], writes=[R[pns[m]]])
                    P.op("act", lambda e, pair=pair, sb_i=sb_i, h=h, dl_=I - J: e.activation(
                        out=stT[sb_i][:].rearrange("p (m i) -> p m i", m=2),
                        in_=pair[:].rearrange("p (m c) -> p m c", m=2)[:, :, 0:128],
                        func=AF.Exp, bias=bias_sb[:, h, dl_:dl_ + 1], scale=1.0),
                        reads=[R[pns[0]], R[pns[1]], R["bias"]], writes=[R[f"stT{sb_i}"]])
                    if J == I:
                        P.op("pool", lambda e, sb_i=sb_i: e.tensor_tensor(
                            out=stT[sb_i][:].rearrange("p (m i) -> p m i", m=2),
                            in0=stT[sb_i][:].rearrange("p (m i) -> p m i", m=2),
                            in1=mask_b[:].unsqueeze(1).to_broadcast([128, 2, 128]), op=ALU.mult),
                            reads=[R[f"stT{sb_i}"], R["mask_b"]], writes=[R[f"stT{sb_i}"]])

                def emit_PV(k):
                    I, J = items[k]
                    sb_i = k % 3
                    po0, pn0, po1, pn1 = acc(I)
                    for m, (po, pon) in enumerate(((po0, pn0), (po1, pn1))):
                        P.op("pe", lambda e, po=po, sb_i=sb_i, J=J, I=I, m=m: e.matmul(
                            out=po[:, 0:129], lhsT=stT[sb_i][:, m * 128:(m + 1) * 128], rhs=dv[:, J, 0:129],
                            start=(J == 0), stop=(J == I)),
                            reads=[R[f"stT{sb_i}"], R["dv"]], writes=[R[pon]])
                    if J == I:
                        combine(I, po0, pn0, po1, pn1)

                emit_S(0)
                for k in range(1, len(items)):
                    emit_S(k)
                    emit_PV(k - 1)
                emit_PV(len(items) - 1)
            chk(5)
            P.barrier()
            for _k in range(16):
                for _n in ("sc", "top", "itop"):
                    if f"{_n}_{_k}" not in R:
                        R[f"{_n}_{_k}"] = P.res(f"{_n}_{_k}")
            for _k in range(8):
                for _n in ("cand", "ctop", "cidx"):
                    if f"{_n}_{_k}" not in R:
                        R[f"{_n}_{_k}"] = P.res(f"{_n}_{_k}")
            SC = [R[f"sc_{k}"] for k in range(16)]
            TOP = [R[f"top_{k}"] for k in range(16)]
            ITOP = [R[f"itop_{k}"] for k in range(16)]
            CAND = [R[f"cand_{k}"] for k in range(8)]
            CTOP = [R[f"ctop_{k}"] for k in range(8)]
            CIDX = [R[f"cidx_{k}"] for k in range(8)]
            def front(i):
                par = i % 2
                x1, xn, eidx, gate = x1s[par], xns[par], eidxs[par], gates[par]
                x1n, xnn, eidxn, gaten = f"x1_{par}", f"xn_{par}", f"eidx_{par}", f"gate_{par}"
                xnb, xnbn = xnbs[par], f"xnb_{par}"
                rows = slice(i * 128, (i + 1) * 128)
                xti = xt[i % 2]
                xr = f"xt{i % 2}"
                P.dma("sp", lambda e, xti=xti, b=b, rows=rows: e.dma_start(out=xti[:], in_=x_d[b, rows, :]),
                      writes=[R[xr]])
                transposes8(cat[:, i, :], R["cat"], pT0, "pT0")
                P.op("act", lambda e: e.activation(out=catT[:], in_=pT0[:], func=AF.Copy), reads=[R["pT0"]], writes=[R["catT"]])
                for half, (pp, pn) in enumerate(((pA, "pA"), (pB, "pB"))):
                    for c in range(8):
                        P.op("pe", lambda e, pp=pp, c=c, half=half: e.matmul(
                            out=pp[:], lhsT=catT[:, c, :], rhs=w_out_b[:, c, half * 512:(half + 1) * 512],
                            start=(c == 0), stop=(c == 7)),
                            reads=[R["catT"], R["w_out_b"]], writes=[R[pn]])
                    P.op("dve", lambda e, pp=pp, half=half, xti=xti: e.tensor_tensor(
                        out=x1[:, half * 512:(half + 1) * 512], in0=pp[:], in1=xti[:, half * 512:(half + 1) * 512], op=ALU.add),
                        reads=[R[pn], R[xr]], writes=[R[x1n]], cost=0.7)
                if debug:
                    P.dma("sp", lambda e, b=b, rows=rows: e.dma_start(out=dbg_d[b, rows, :], in_=x1[:]),
                          reads=[R[x1n]], chan=R[x1n])
                rms_stats(x1[:], [R[x1n]], D, 8)
                P.op("dve", lambda e: e.scalar_tensor_tensor(out=xnb[:], in0=x1[:], scalar=st[:, 8:9], in1=gffn[:],
                                                             op0=ALU.mult, op1=ALU.mult),
                     reads=[R[x1n], R["st"], R["gffn"]], writes=[R[xnbn]], cost=1.2)
                transposes8(xnb, R[xnbn], pT1, "pT1")
                P.op("act", lambda e: e.activation(out=xnT[:], in_=pT1[:], func=AF.Copy), reads=[R["pT1"]], writes=[R["xnT"]])
                for g4 in range(4):
                    pp, pn = (pC, "pC") if g4 % 2 == 0 else (pD, "pD")
                    for q4 in range(4):
                        cb = g4 * 4 + q4
                        for c in range(8):
                            P.op("pe", lambda e, pp=pp, q4=q4, cb=cb, c=c: e.matmul(
                                out=pp[:, q4 * 128:(q4 + 1) * 128], lhsT=wq_b[:, c, cb * 128:(cb + 1) * 128], rhs=xnT[:, c, :],
                                start=(c == 0), stop=(c == 7)),
                                reads=[R["wq_b"], R["xnT"]], writes=[R[pn]])
                    P.op("act", lambda e, pp=pp, g4=g4: e.activation(
                        out=qT_sb[:, g4 * 4:(g4 + 1) * 4, :], in_=pp[:].rearrange("p (a b) -> p a b", b=128), func=AF.Copy),
                        reads=[R[pn]], writes=[R["qT_sb"]])
                for g4 in range(4):
                    pp, pn = ((pA, "pA"), (pB, "pB"), (pC, "pC"), (pD, "pD"))[g4]
                    for q4 in range(4):
                        cb = g4 * 4 + q4
                        P.op("pe", lambda e, pp=pp, q4=q4, cb=cb: e.matmul(
                            out=pp[:, q4 * 128:(q4 + 1) * 128], lhsT=qT_sb[:, cb, :], rhs=skT_b[:, cb, :], start=True, stop=True),
                            reads=[R["qT_sb"], R["skT_b"]], writes=[R[pn]])
                    P.op("act", lambda e, pp=pp, g4=g4: e.activation(
                        out=sc[:, g4 * 4:(g4 + 1) * 4, :], in_=pp[:].rearrange("p (a b) -> p a b", b=128), func=AF.Copy),
                        reads=[R[pn]], writes=SC[g4 * 4:(g4 + 1) * 4])
                for cb in range(16):
                    P.op("dve", lambda e, cb=cb: e.max(out=top[:, cb, 0:8], in_=sc[:, cb, :]), reads=[SC[cb]], writes=[TOP[cb]])
                for cb in range(16):
                    P.op("dve", lambda e, cb=cb: e.max_index(out=itop[:, cb, 0:8], in_max=top[:, cb, 0:8], in_values=sc[:, cb, :]),
                         reads=[SC[cb], TOP[cb]], writes=[ITOP[cb]])
                for cb in range(16):
                    P.op("dve", lambda e, cb=cb: e.match_replace(out=sc[:, cb, :], in_to_replace=top[:, cb, 0:8],
                                                                 in_values=sc[:, cb, :], imm_value=-1e30),
                         reads=[TOP[cb]], writes=[SC[cb]])
                for cb in range(16):
                    P.op("dve", lambda e, cb=cb: e.max(out=top[:, cb, 8:16], in_=sc[:, cb, :]), reads=[SC[cb]], writes=[TOP[cb]])
                for cb in range(16):
                    P.op("dve", lambda e, cb=cb: e.max_index(out=itop[:, cb, 8:16], in_max=top[:, cb, 8:16], in_values=sc[:, cb, :]),
                         reads=[SC[cb], TOP[cb]], writes=[ITOP[cb]])
                P.op("dve", lambda e: e.tensor_copy(out=itopf[:], in_=itop[:]), reads=ITOP, writes=[R["itopf"]])
                top4 = top[:].rearrange("p (h two) r -> p h two r", two=2)
                itf4 = itopf[:].rearrange("p (h two) r -> p h two r", two=2)
                P.op("dve", lambda e: e.tensor_tensor(
                    out=cand[:].rearrange("p h (a b) -> p h a b", b=16),
                    in0=top4[:, :, 0, :].unsqueeze(3).to_broadcast([128, 8, 16, 16]),
                    in1=top4[:, :, 1, :].unsqueeze(2).to_broadcast([128, 8, 16, 16]), op=ALU.add),
                    reads=TOP, writes=CAND, cost=2.3)
                for hh in range(8):
                    P.op("dve", lambda e, hh=hh: e.max(out=ctop[:, hh, 0:8], in_=cand[:, hh, :]), reads=[CAND[hh]], writes=[CTOP[hh]])
                for hh in range(8):
                    P.op("dve", lambda e, hh=hh: e.max_index(out=cidx[:, hh, 0:8], in_max=ctop[:, hh, 0:8], in_values=cand[:, hh, :]),
                         reads=[CAND[hh], CTOP[hh]], writes=[CIDX[hh]])
                for hh in range(8):
                    P.op("dve", lambda e, hh=hh: e.match_replace(out=cand[:, hh, :], in_to_replace=ctop[:, hh, 0:8],
                                                                 in_values=cand[:, hh, :], imm_value=-1e30),
                         reads=[CTOP[hh]], writes=[CAND[hh]])
                for hh in range(8):
                    P.op("dve", lambda e, hh=hh: e.max(out=ctop[:, hh, 8:16], in_=cand[:, hh, :]), reads=[CAND[hh]], writes=[CTOP[hh]])
                for hh in range(8):
                    P.op("dve", lambda e, hh=hh: e.max_index(out=cidx[:, hh, 8:16], in_max=ctop[:, hh, 8:16], in_values=cand[:, hh, :]),
                         reads=[CAND[hh], CTOP[hh]], writes=[CIDX[hh]])
                P.op("dve", lambda e: e.tensor_copy(out=cidxf[:], in_=cidx[:].rearrange("p h k -> p (h k)")),
                     reads=CIDX, writes=[R["cidxf"]])
                P.op("dve", lambda e: e.tensor_tensor(
                    out=eq[:].rearrange("p h (k r) -> p (h k) r", r=16),
                    in0=cidxf[:].unsqueeze(2).to_broadcast([128, 128, 16]),
                    in1=thr[:].unsqueeze(1).to_broadcast([128, 128, 16]), op=ALU.is_ge),
                    reads=[R["cidxf"], R["thr"]], writes=CAND, cost=2.3)
                P.op("dve", lambda e: e.tensor_reduce(
                    out=r12f[:, 0, :], in_=eq[:].rearrange("p h (k r) -> p (h k) r", r=16), axis=AX.X, op=ALU.add),
                    reads=CAND, writes=[R["r12f"]], cost=2.3)
                P.op("dve", lambda e: e.scalar_tensor_tensor(out=r12f[:, 1, :], in0=r12f[:, 0, :], scalar=-16.0, in1=cidxf[:],
                                                             op0=ALU.mult, op1=ALU.add),
                     reads=[R["r12f"], R["cidxf"]], writes=[R["r12f"]])
                for w in range(2):
                    P.op("dve", lambda e, w=w: e.tensor_tensor(
                        out=eq[:].rearrange("p h (k r) -> p h k r", r=16),
                        in0=iota16[:].unsqueeze(1).unsqueeze(1).to_broadcast([128, 8, 16, 16]),
                        in1=r12f[:, w, :].rearrange("p (h k) -> p h k", k=16).unsqueeze(3).to_broadcast([128, 8, 16, 16]),
                        op=ALU.is_equal), reads=[R["iota16"], R["r12f"]], writes=CAND, cost=2.3)
                    P.op("dve", lambda e, w=w: e.tensor_tensor(
                        out=eq[:].rearrange("p h (k r) -> p h k r", r=16),
                        in0=eq[:].rearrange("p h (k r) -> p h k r", r=16),
                        in1=itf4[:, :, w, :].unsqueeze(2).to_broadcast([128, 8, 16, 16]), op=ALU.mult),
                        reads=[R["itopf"]], writes=CAND, cost=2.3)
                    P.op("dve", lambda e, w=w: e.tensor_reduce(
                        out=i12f[:, w, :], in_=eq[:].rearrange("p h (k r) -> p (h k) r", r=16), axis=AX.X, op=ALU.add),
                        reads=CAND, writes=[R["i12f"]], cost=2.3)
                P.op("dve", lambda e: e.scalar_tensor_tensor(out=ef[:], in0=i12f[:, 0, :], scalar=128.0, in1=i12f[:, 1, :],
                                                             op0=ALU.mult, op1=ALU.add), reads=[R["i12f"]], writes=[R["ef"]])
                P.op("dve", lambda e: e.tensor_scalar(out=ef[:], in0=ef[:], scalar1=0.0, scalar2=16383.0, op0=ALU.max, op1=ALU.min),
                 reads=[R["ef"]], writes=[R["ef"]])
                P.op("dve", lambda e: e.tensor_copy(out=eidx[:], in_=ef[:]), reads=[R["ef"]], writes=[R[eidxn]])
                P.op("dve", lambda e: e.tensor_scalar(out=gz[:, 0:8], in0=ctop[:, :, 0], scalar1=-1.0, scalar2=None, op0=ALU.mult),
                     reads=CTOP, writes=[R["gz"]])
                for hh in range(8):
                    P.op("act", lambda e, hh=hh: e.activation(out=gate[:, hh * 16:(hh + 1) * 16], in_=ctop[:, hh, :], func=AF.Exp,
                                                              bias=gz[:, hh:hh + 1], scale=1.0),
                         reads=[CTOP[hh], R["gz"]], writes=[R[gaten]])
                P.op("dve", lambda e: e.tensor_reduce(out=gz[:, 8:16], in_=gate[:].rearrange("p (h k) -> p h k", k=16),
                                                      axis=AX.X, op=ALU.add), reads=[R[gaten]], writes=[R["gz"]])
                P.op("dve", lambda e: e.reciprocal(out=gz[:, 8:16], in_=gz[:, 8:16]), reads=[R["gz"]], writes=[R["gz"]])
                P.op("dve", lambda e: e.tensor_tensor(
                    out=gate[:].rearrange("p (h k) -> p h k", k=16), in0=gate[:].rearrange("p (h k) -> p h k", k=16),
                    in1=gz[:, 8:16].unsqueeze(2).to_broadcast([128, 8, 16]), op=ALU.mult),
                    reads=[R[gaten], R["gz"]], writes=[R[gaten]])

            def stream(i, filler):
                par = i % 2
                x1, xn, eidx, gate = x1s[par], xns[par], eidxs[par], gates[par]
                x1n, xnn, eidxn, gaten = f"x1_{par}", f"xn_{par}", f"eidx_{par}", f"gate_{par}"
                xnb, xnbn = xnbs[par], f"xnb_{par}"
                rows = slice(i * 128, (i + 1) * 128)
                P.op("dve", lambda e: e.memset(actv[:], 0.0), writes=[R[f"actv{i}"] for i in range(8)])
                scr = [R["scr0"], R["scr1"], R["scr2"], R["scr3"]]
                for s in range(128):
                    gb = s % NG
                    di = s % 2
                    s8 = s % 8
                    P.dma("pool", lambda e, s=s, gb=gb: e.indirect_dma_start(
                        out=gbuf[gb][:], out_offset=None, in_=uv_d,
                        in_offset=bass.IndirectOffsetOnAxis(ap=eidx[:, s:s + 1], axis=0)),
                        reads=[R[eidxn]] + scr, writes=[R[f"gbuf{gb}"]])
                    P.op("dve", lambda e, s=s, gb=gb, di=di: e.scalar_tensor_tensor(
                        out=jkb[di][:], in0=gbuf[gb][:, 0:D], scalar=1.0, in1=xnb[:], op0=ALU.mult, op1=ALU.mult,
                        accum_out=actv[:, s:s + 1]),
                        reads=[R[f"gbuf{gb}"], R[xnbn]], writes=[R[f"jkb{di}"], R[f"actv{s8}"]])
                    P.op("act", lambda e, s=s: e.activation(out=cf[:, s:s + 1], in_=actv[:, s:s + 1], func=AF.Gelu),
                         reads=[R[f"actv{s8}"]], writes=[R[f"cf{s8}"]])
                    P.op("act", lambda e, s=s: e.activation(out=cf2[:, s:s + 1], in_=cf[:, s:s + 1], func=AF.Copy,
                                                            scale=gate[:, s:s + 1]),
                         reads=[R[f"cf{s8}"], R[gaten]], writes=[R[f"cf{s8}"]])
                    P.op("act", lambda e, s=s, di=di: e.activation(out=dg[di][:], in_=ident_f[:], func=AF.Copy,
                                                                   scale=cf2[:, s:s + 1]),
                         reads=[R["ident_f"], R[f"cf{s8}"]], writes=[R[f"dg{di}"]])
                    for half, (pp, pn) in enumerate(((pE, "pE"), (pF, "pF"))):
                        P.op("pe", lambda e, pp=pp, di=di, gb=gb, half=half, s=s: e.matmul(
                            out=pp[:], lhsT=dg[di][:], rhs=gbuf[gb][:, D + half * 512:D + (half + 1) * 512],
                            start=(s == 0), stop=(s == 127)),
                            reads=[R[f"dg{di}"], R[f"gbuf{gb}"]], writes=[R[pn]])
                    filler()

            def tail(i):
                par = i % 2
                x1, xn, eidx, gate = x1s[par], xns[par], eidxs[par], gates[par]
                x1n, xnn, eidxn, gaten = f"x1_{par}", f"xn_{par}", f"eidx_{par}", f"gate_{par}"
                xnb, xnbn = xnbs[par], f"xnb_{par}"
                rows = slice(i * 128, (i + 1) * 128)
                for half, (pp, pn) in enumerate(((pE, "pE"), (pF, "pF"))):
                    P.op("dve", lambda e, pp=pp, half=half: e.tensor_tensor(
                        out=x2[:, half * 512:(half + 1) * 512], in0=pp[:], in1=x1[:, half * 512:(half + 1) * 512], op=ALU.add),
                        reads=[R[pn], R[x1n]], writes=[R["x2"]])
                rms_stats(x2[:], [R["x2"]], D, 9)
                P.op("dve", lambda e: e.scalar_tensor_tensor(out=ob[:], in0=x2[:], scalar=st[:, 9:10], in1=gfin[:],
                                                         op0=ALU.mult, op1=ALU.mult),
                 reads=[R["st"], R["gfin"]], writes=[R["x2"]])
                P.dma("sp", lambda e, b=b, rows=rows: e.dma_start(out=out_d[b, rows, :], in_=ob[:]),
                  reads=[R["ob"]], chan=R["ob"])

            P.capture = []
            front(0)
            cap = P.capture
            P.capture = None
            P.replay(cap)
            for i in range(NT):
                if i + 1 < NT:
                    P.capture = []
                    front(i + 1)
                    cap = P.capture
                    P.capture = None
                else:
                    cap = []
                pos = [0]
                tot = sum((it[5] or 0.0) for it in cap if it[0] == "op") + 1e-6
                budget = tot / 116.0
                spent = [0.0]
                slot = [0]

                def filler(cap=cap, pos=pos, budget=budget, spent=spent, slot=slot):
                    slot[0] += 1
                    target = budget * slot[0]
                    j = pos[0]
                    n0 = j
                    while j < len(cap) and (spent[0] < target) and (j - n0) < 24:
                        if cap[j][0] == "op":
                            spent[0] += (cap[j][5] or 0.0)
                        j += 1
                    P.replay(cap[pos[0]:j])
                    pos[0] = j

                stream(i, filler)
                P.replay(cap[pos[0]:])
                tail(i)


    def chk(level):
        if stop == level:
            raise _Stop()

    try:
        _main_body(chk)
    except _Stop:
        pass
    fin = ([R["ob"]] if R["ob"].r else []) + ([R["x1"], R["x1_b"]] if debug else [])
    P.emit(final_waits=fin)
    P.close()
    return nc, consts


def make_in_maps(inputs, consts, bpc=BPC, ncores=NCORES):
    f = np.float32
    x = np.ascontiguousarray(inputs["x"], dtype=f)
    common = {
        "w_in": np.ascontiguousarray(inputs["w_in"][0]),
        "w_out": np.ascontiguousarray(inputs["w_out"][0]),
        "wq": np.ascontiguousarray(inputs["peer_wq"][0]),
        "skT": np.ascontiguousarray(np.transpose(inputs["peer_subkeys"][0].reshape(16, 128, 128), (2, 0, 1))),
        "u": np.ascontiguousarray(inputs["peer_u"][0]),
        "v": np.ascontiguousarray(inputs["peer_v"][0]),
        "gmix_col": np.ascontiguousarray(inputs["norm_mix_g"][0].reshape(8, 128).T),
        "gffn_b": np.ascontiguousarray(np.broadcast_to(inputs["norm_ffn_g"][0][None], (128, D))),
        "gfin_b": np.ascontiguousarray(np.broadcast_to(inputs["final_norm_g"][None], (128, D))),
        "gsub_b": np.ascontiguousarray(np.broadcast_to(inputs["diff_subln_g"][0][None], (128, 128))),
        "dl_b": np.ascontiguousarray(np.broadcast_to(inputs["diff_lambda"][0][None], (128, 4, 64))),
    }
    common = {k: np.asarray(a, dtype=f) for k, a in common.items()}
    common.update(consts)
    maps = []
    for c in range(ncores):
        m = dict(common)
        m["x"] = np.ascontiguousarray(x[c * bpc:(c + 1) * bpc])
        maps.append(m)
    return maps


_CACHE = {}


def kernel(**inputs):
    inputs = {k: np.asarray(v) for k, v in inputs.items()}
    if "nc" not in _CACHE:
        _CACHE["nc"] = build_program()
    nc, consts = _CACHE["nc"]
    in_maps = make_in_maps(inputs, consts)
    res = run_bass_kernel_spmd(nc, in_maps, core_ids=list(range(NCORES)))
    out = np.concatenate([r["out"] for r in res.results], axis=0)
    return out.astype(np.float32)
```

```python
import os
import numpy as np
import concourse.bass as bass
import concourse.mybir as mybir
from concourse.bass_utils import run_bass_kernel_spmd

F32 = mybir.dt.float32
BF16 = mybir.dt.bfloat16
I32 = mybir.dt.int32
U32 = mybir.dt.uint32
ALU = mybir.AluOpType
AF = mybir.ActivationFunctionType
AX = mybir.AxisListType

ENGS = ("pe", "act", "dve", "pool", "sp")
NCORES = 8
BPC = 2
T = 2048
D = 1024
NT = T // 128
RMS_EPS = 1e-6
LAMBDA_INIT = 0.8 - 0.6 * float(np.exp(-0.3 * 0))


class Res:
    __slots__ = ("name", "w", "r", "dsem", "dcnt", "excl")

    def __init__(self, name):
        self.name = name
        self.excl = False
        self.w = None
        self.r = []
        self.dsem = None
        self.dcnt = 0


class Prog:
    def __init__(self, nc, same_engine_sync=("act", "dve", "pool")):
        self.nc = nc
        self.ops = {e: [] for e in ENGS}
        self.waited = {e: {} for e in ENGS}
        self.same = set(same_engine_sync)
        self.ctx = []
        self.esem = {}
        self.pending = {e: [] for e in ENGS}
        self.all_res = []
        self.capture = None

    def res(self, name):
        r = Res(name)
        self.all_res.append(r)
        return r

    def barrier(self):
        tks = []
        for r in self.all_res:
            if r.w is not None:
                tks.append(r.w)
            tks.extend(r.r)
        for e in ENGS:
            self.pending[e] = list(tks)

    def sb(self, name, shape, dt):
        g = self.nc.sbuf_tensor(name, list(shape), dt)
        t = g.__enter__()
        self.ctx.append(g)
        return t

    def ps(self, name, shape, dt):
        g = self.nc.psum_tensor(name, list(shape), dt)
        t = g.__enter__()
        self.ctx.append(g)
        return t

    def sem(self, name):
        g = self.nc.semaphore(name)
        s = g.__enter__()
        self.ctx.append(g)
        return s

    def _need(self, eng, tk, waits):
        if tk is None:
            return
        if tk[0] == "e":
            _, e2, seq = tk
            if e2 == eng and eng not in self.same:
                return
            key = ("e", e2)
            if self.waited[eng].get(key, 0) >= seq:
                return
            self.waited[eng][key] = seq
            self.ops[e2][seq - 1]["signal"] = True
            waits.append(tk)
        else:
            _, res, val = tk
            key = ("d", id(res))
            if self.waited[eng].get(key, 0) >= val:
                return
            self.waited[eng][key] = val
            waits.append(tk)

    def _deps(self, eng, reads, writes):
        waits = []
        if self.pending[eng]:
            for tk in self.pending[eng]:
                self._need(eng, tk, waits)
            self.pending[eng] = []
        for r in reads:
            self._need(eng, r.w, waits)
        for w in writes:
            self._need(eng, w.w, waits)
            for t in w.r:
                self._need(eng, t, waits)
        return waits

    def replay(self, items):
        for kind, eng, fn, reads, writes, chan in items:
            if kind == "op":
                self.op(eng, fn, reads, writes)
            else:
                self.dma(eng, fn, reads, writes, chan)

    def op(self, eng, fn, reads=(), writes=(), cost=None):
        if self.capture is not None:
            if cost is None:
                cost = 0.3 if eng == "dve" else 0.0
            self.capture.append(("op", eng, fn, list(reads), list(writes), cost))
            return None
        xr = [r for r in reads if r.excl]
        if xr:
            reads = [r for r in reads if not r.excl]
            writes = list(writes) + [r for r in xr if r not in writes]
        waits = self._deps(eng, reads, writes)
        self.ops[eng].append(dict(fn=fn, waits=waits, signal=False, dma=None))
        tk = ("e", eng, len(self.ops[eng]))
        for r in reads:
            r.r.append(tk)
        for w in writes:
            w.w = tk
            w.r = []
        return tk

    def dma(self, eng, fn, reads=(), writes=(), chan=None):
        if self.capture is not None:
            self.capture.append(("dma", eng, fn, list(reads), list(writes), chan))
            return None
        waits = self._deps(eng, reads, writes)
        if chan is None:
            chan = writes[0] if writes else reads[0]
        if chan.dsem is None:
            chan.dsem = self.sem("d_" + chan.name)
        chan.dcnt += 16
        self.ops[eng].append(dict(fn=fn, waits=waits, signal=False, dma=(chan, chan.dcnt)))
        tk = ("d", chan, chan.dcnt)
        for r in reads:
            r.r.append(tk)
        for w in writes:
            w.w = tk
            w.r = []
        return tk

    def emit(self, final_waits=()):
        nc = self.nc
        for e in ENGS:
            self.esem[e] = self.sem("e_" + e)
        fw = []
        for r in final_waits:
            self._need("sp", r.w, fw)
            for t in r.r:
                self._need("sp", t, fw)
        self.ops["sp"].append(dict(fn=None, waits=fw, signal=False, dma=None))
        val = {}
        for e in ENGS:
            c = 0
            v = []
            for o in self.ops[e]:
                if o["signal"]:
                    c += 1
                v.append(c)
            val[e] = v

        def run(e, eo):
            for o in self.ops[e]:
                for tk in o["waits"]:
                    if tk[0] == "e":
                        eo.wait_ge(self.esem[tk[1]], val[tk[1]][tk[2] - 1])
                    else:
                        eo.wait_ge(tk[1].dsem, tk[2])
                if o["fn"] is None:
                    continue
                ins = o["fn"](eo)
                if o["dma"] is not None:
                    ins.then_inc(o["dma"][0].dsem, 16)
                elif o["signal"]:
                    ins.then_inc(self.esem[e], 1)

        with nc.Block() as block:
            @block.sync
            def _(eo):
                run("sp", eo)

            @block.tensor
            def _(eo):
                run("pe", eo)

            @block.scalar
            def _(eo):
                run("act", eo)

            @block.vector
            def _(eo):
                run("dve", eo)

            @block.gpsimd
            def _(eo):
                run("pool", eo)

    def close(self):
        for g in reversed(self.ctx):
            g.__exit__(None, None, None)
        self.ctx = []


def host_consts():
    f = np.float32
    H = 4
    C = 128
    idx = np.arange(C, dtype=f)
    log_gamma = np.log1p(-np.exp2(-5.0 - np.arange(H, dtype=f))).astype(f)
    rel = idx[None, :] - idx[:, None]
    DT = np.where(rel >= 0, np.exp(log_gamma[:, None, None] * np.maximum(rel, 0.0)[None]), 0.0)
    DT = (DT * (128.0 ** -0.5)).astype(f)
    cross = np.exp(log_gamma[:, None] * (idx + 1.0)).astype(f)
    kdec = (np.exp(log_gamma[:, None] * (C - 1.0 - idx)) * (128.0 ** -0.5)).astype(f)
    cdec = np.exp(log_gamma * C).astype(f)
    slopes = np.exp2(-8.0 * (np.arange(H, dtype=f) + 1.0) / H).astype(f)
    jp = idx
    bias = np.zeros((C, H, 16), f)
    for h in range(H):
        for dlt in range(16):
            bias[:, h, dlt] = slopes[h] * (jp - 127.0) - slopes[h] * 128.0 * dlt
    c = {}
    c["c_ident"] = np.eye(128, dtype=f)
    c["c_DT"] = np.ascontiguousarray(DT.transpose(1, 0, 2))
    c["c_cross"] = np.ascontiguousarray(np.broadcast_to(cross[None], (128, H, C))).astype(f)
    c["c_kdec"] = np.ascontiguousarray(kdec.T)
    c["c_bias"] = bias
    c["c_mask"] = (idx[:, None] <= idx[None, :]).astype(f)
    c["c_iota16"] = np.ascontiguousarray(np.broadcast_to(np.arange(16, dtype=f)[None], (128, 16)))
    c["c_thr"] = np.ascontiguousarray(np.broadcast_to((16.0 * (np.arange(16, dtype=f) + 1.0))[None], (128, 16)))
    return c, [float(x) for x in cdec]


class _Stop(Exception):
    pass


def build_program(debug=False, bpc=BPC, peer=True, stop=None):
    consts, cdec = host_consts()
    nc = bass.Bass("TRN2", target_bir_lowering=False)
    dr = {}

    def din(name, shape, dt=F32):
        dr[name] = nc.dram_tensor(name, list(shape), dt, kind="ExternalInput").ap()
        return dr[name]

    x_d = din("x", [bpc, T, D])
    w_in_d = din("w_in", [D, 3584])
    w_out_d = din("w_out", [D, D])
    wq_d = din("wq", [D, 2048])
    skT_d = din("skT", [128, 16, 128])
    u_d = din("u", [16384, D])
    v_d = din("v", [16384, D])
    gmix_d = din("gmix_col", [128, 8])
    gffn_d = din("gffn_b", [128, D])
    gfin_d = din("gfin_b", [128, D])
    gsub_d = din("gsub_b", [128, 128])
    dl_d = din("dl_b", [128, 4, 64])
    for k, a in consts.items():
        din(k, a.shape)
    uv_d = nc.dram_tensor("uv_scr", [16384, 2 * D], BF16, kind="Internal").ap()
    out_d = nc.dram_tensor("out", [bpc, T, D], F32, kind="ExternalOutput").ap()
    if debug:
        dbg_d = nc.dram_tensor("dbg", [bpc, T, D], F32, kind="ExternalOutput").ap()

    P = Prog(nc)
    R = {}

    def sbt(name, shape, dt, nres=1):
        t = P.sb(name, shape, dt)
        R[name] = P.res(name)
        return t

    UW = 22944
    U = P.sb("U", [128, UW], F32)
    uoff = {"o": 0}

    def carve(name, shape, dt):
        n = int(np.prod(shape[1:]))
        esz = 2 if dt == BF16 else 4
        words = (n * esz + 3) // 4
        words += words % 2
        assert uoff["o"] + words <= UW, (name, uoff["o"], words)
        ap = U[:, uoff["o"]:uoff["o"] + words]
        uoff["o"] += words
        if dt != F32:
            ap = ap.bitcast(dt)
        ap = ap[:, 0:n]
        if len(shape) == 3:
            ap = ap.rearrange("p (a b) -> p a b", b=shape[2])
        R[name] = P.res(name)
        return ap

    ident_f = sbt("ident_f", [128, 128], F32)
    ident_b = sbt("ident_b", [128, 128], BF16)
    DT_sb = sbt("DT", [128, 4, 128], F32)
    cross_sb = sbt("cross", [128, 4, 128], F32)
    kdec_sb = sbt("kdec", [128, 4], F32)
    bias_sb = sbt("bias", [128, 4, 16], F32)
    mask_sb = sbt("mask", [128, 128], F32)
    iota16 = sbt("iota16", [128, 16], F32)
    thr = sbt("thr", [128, 16], F32)
    gmix = sbt("gmix", [128, 8], F32)
    gffn = sbt("gffn", [128, D], F32)
    gfin = sbt("gfin", [128, D], F32)
    gsub = sbt("gsub", [128, 128], F32)
    dl = sbt("dl", [128, 4, 64], F32)
    lamt = sbt("lamt", [128, 8], F32)
    w_out_b = sbt("w_out_b", [128, 8, D], BF16)
    wq_b = sbt("wq_b", [128, 8, 2048], BF16)
    skT_b = sbt("skT_b", [128, 16, 128], BF16)

    def load(dst, src, name, eng="sp"):
        P.dma(eng, lambda e: e.dma_start(out=dst, in_=src), writes=[R[name]])

    load(ident_f[:], dr["c_ident"], "ident_f")
    load(DT_sb[:], dr["c_DT"], "DT")
    load(cross_sb[:], dr["c_cross"], "cross")
    load(kdec_sb[:], dr["c_kdec"], "kdec")
    load(bias_sb[:], dr["c_bias"], "bias")
    load(mask_sb[:], dr["c_mask"], "mask")
    load(iota16[:], dr["c_iota16"], "iota16")
    load(thr[:], dr["c_thr"], "thr")
    load(gmix[:], gmix_d, "gmix")
    load(gffn[:], gffn_d, "gffn")
    load(gfin[:], gfin_d, "gfin")
    load(gsub[:], gsub_d, "gsub")
    load(dl[:], dl_d, "dl")
    load(w_out_b[:], w_out_d.rearrange("(c p) n -> p c n", p=128), "w_out_b", eng="pool")
    for c in range(8):
        P.dma("pool", lambda e, c=c: e.dma_start(out=wq_b[:, c, :], in_=wq_d[c * 128:(c + 1) * 128, :]),
              writes=[R["wq_b"]])
    load(skT_b[:], skT_d, "skT_b", eng="pool")
    P.op("dve", lambda e: e.tensor_copy(out=ident_b[:], in_=ident_f[:]), reads=[R["ident_f"]], writes=[R["ident_b"]])

    junk64 = sbt("junk64", [128, 64], F32)
    P.op("dve", lambda e: e.memset(lamt[:], 0.0), writes=[R["lamt"]])
    P.op("dve", lambda e: e.scalar_tensor_tensor(out=junk64[:], in0=dl[:, 0, :], scalar=1.0, in1=dl[:, 1, :],
                                                 op0=ALU.mult, op1=ALU.mult, accum_out=lamt[:, 0:1]),
         reads=[R["dl"]], writes=[R["junk64"], R["lamt"]])
    P.op("dve", lambda e: e.scalar_tensor_tensor(out=junk64[:], in0=dl[:, 2, :], scalar=1.0, in1=dl[:, 3, :],
                                                 op0=ALU.mult, op1=ALU.mult, accum_out=lamt[:, 1:2]),
         reads=[R["dl"]], writes=[R["junk64"], R["lamt"]])
    P.op("act", lambda e: e.activation(out=lamt[:, 2:4], in_=lamt[:, 0:2], func=AF.Exp),
         reads=[R["lamt"]], writes=[R["lamt"]])
    P.op("dve", lambda e: e.tensor_tensor(out=lamt[:, 4:5], in0=lamt[:, 3:4], in1=lamt[:, 2:3], op=ALU.subtract),
         reads=[R["lamt"]], writes=[R["lamt"]])
    P.op("dve", lambda e: e.tensor_scalar(out=lamt[:, 5:6], in0=lamt[:, 4:5], scalar1=-LAMBDA_INIT, scalar2=None,
                                          op0=ALU.add), reads=[R["lamt"]], writes=[R["lamt"]])
    neglam = lamt[:, 5:6]

    cat = sbt("cat", [128, NT, D], BF16)
    xt = [sbt(f"xt{i}", [128, D], F32) for i in range(2)]
    xb = sbt("xb", [128, D], BF16)
    junk = sbt("junk", [128, D], F32)
    st = sbt("st", [128, 16], F32)
    S_f = sbt("S_f", [128, 128], F32)
    S_b = sbt("S_b", [128, 128], BF16)
    stT = [sbt(f"stT{i}", [128, 256], BF16) for i in range(3)]
    o_sb = sbt("o_sb", [128, 128], F32)
    t1 = sbt("t1", [128, 128], F32)
    mask_b = sbt("mask_b", [128, 128], BF16)
    uoff["o"] = 0
    hT = carve("hT", [128, 8, T], BF16)
    wtok = carve("wtok", [128, 8, 512], BF16)
    wfeat = carve("wfeat", [128, 8, 512], BF16)
    qTr = carve("qTr", [128, T], BF16)
    qTrc = carve("qTrc", [128, T], BF16)
    kTr = carve("kTr", [128, T], BF16)
    qTd = carve("qTd", [128, T], BF16)
    kTd = carve("kTd", [128, T], BF16)
    kd = carve("kd", [128, NT, 128], BF16)
    rv = carve("rv", [128, NT, 128], BF16)
    sg = carve("sg", [128, NT, 128], BF16)
    dv = carve("dv", [128, NT, 132], BF16)
    uoff["o"] = 0
    catT = carve("catT", [128, 8, 128], BF16)
    x1 = carve("x1", [128, D], F32)
    xn = None
    xnT = carve("xnT", [128, 8, 128], BF16)
    qT_sb = carve("qT_sb", [128, 16, 128], BF16)
    sc = carve("sc", [128, 16, 128], F32)
    top = carve("top", [128, 16, 16], F32)
    itop = carve("itop", [128, 16, 16], U32)
    itopf = carve("itopf", [128, 16, 16], F32)
    cand = carve("cand", [128, 8, 256], F32)
    ctop = carve("ctop", [128, 8, 16], F32)
    cidx = carve("cidx", [128, 8, 16], U32)
    cidxf = carve("cidxf", [128, 128], F32)
    r12f = carve("r12f", [128, 2, 128], F32)
    eq = cand
    R["eq"] = R["cand"]
    i12f = carve("i12f", [128, 2, 128], F32)
    ef = carve("ef", [128, 128], F32)
    eidx = carve("eidx", [128, 128], I32)
    gate = carve("gate", [128, 128], F32)
    gz = carve("gz", [128, 16], F32)
    actv = carve("actv", [128, 128], F32)
    coef = carve("coef", [128, 128], F32)
    NG = 9
    gbuf = [carve(f"gbuf{i}", [128, 2 * D], BF16) for i in range(NG)]
    dg = [carve(f"dg{i}", [128, 128], BF16) for i in range(2)]
    x2 = carve("x2", [128, D], F32)
    ob = x2
    R["ob"] = R["x2"]
    jkb = [carve("jkb0", [128, D], BF16)] * 2
    R["jkb1"] = R["jkb0"]
    x1s = [x1, carve("x1_b", [128, D], F32)]
    xns = [None, None]
    eidxs = [eidx, carve("eidx_b", [128, 128], I32)]
    gates = [gate, carve("gate_b", [128, 128], F32)]
    xnbs = [carve("xnb_0", [128, D], BF16), carve("xnb_1", [128, D], BF16)]
    for nm in ("x1", "eidx", "gate"):
        R[nm + "_0"] = R[nm]
        R[nm + "_1"] = R[nm + "_b"]
    print("P34 union words used", uoff["o"], "of", UW)
    cf = carve("cf", [128, 128], F32)
    cf2 = carve("cf2", [128, 128], F32)
    for i in range(8):
        R[f"actv{i}"] = P.res(f"actv{i}")
        R[f"cf{i}"] = P.res(f"cf{i}")

    pA = P.ps("pA", [128, 512], F32); R["pA"] = P.res("pA"); R["pA"].excl = True
    pB = P.ps("pB", [128, 512], F32); R["pB"] = P.res("pB"); R["pB"].excl = True
    pCD = P.ps("pCD", [128, 1024], F32)
    pC = pCD[:, 0:512]; R["pC"] = P.res("pC"); R["pC"].excl = True
    pD = pCD[:, 512:1024]; R["pD"] = P.res("pD"); R["pD"].excl = True
    pE = P.ps("pE", [128, 512], F32); R["pE"] = P.res("pE"); R["pE"].excl = True
    pF = P.ps("pF", [128, 512], F32); R["pF"] = P.res("pF"); R["pF"].excl = True
    pT01 = P.ps("pT01", [128, 2, 1024], BF16)
    pT0 = pT01[:, 0, :].rearrange("p (c t) -> p c t", t=128); R["pT0"] = P.res("pT0"); R["pT0"].excl = True
    pT1 = pT01[:, 1, :].rearrange("p (c t) -> p c t", t=128); R["pT1"] = P.res("pT1"); R["pT1"].excl = True
    pT01f = pT01[:].rearrange("p a b -> p (a b)").bitcast(F32)

    P.op("dve", lambda e: e.tensor_copy(out=mask_b[:], in_=mask_sb[:]), reads=[R["mask"]], writes=[R["mask_b"]])

    uoff["o"] = 0
    NSTG = 4
    stg = [carve(f"stg{i}", [128, 8, D], BF16) for i in range(NSTG)]
    for i in range(NSTG):
        R[f"scr{i}"] = P.res(f"scr{i}")
    k = 0
    for src, dst in ((u_d, uv_d[:, 0:D]), (v_d, uv_d[:, D:2 * D])):
        srcv = src.rearrange("(p k) d -> p k d", p=128)
        dstv = dst.rearrange("(p k) d -> p k d", p=128)
        for c in range(16):
            sb_i = k % NSTG
            k += 1
            P.dma("pool", lambda e, srcv=srcv, c=c, sb_i=sb_i: e.dma_start(out=stg[sb_i][:], in_=srcv[:, c * 8:(c + 1) * 8, :]),
                  writes=[R[f"stg{sb_i}"]])
            P.dma("sp", lambda e, dstv=dstv, c=c, sb_i=sb_i: e.dma_start(out=dstv[:, c * 8:(c + 1) * 8, :], in_=stg[sb_i][:]),
                  reads=[R[f"stg{sb_i}"]], writes=[R[f"scr{sb_i}"]])

    def rms_stats(src_ap, src_res, ncols, col):
        jk = junk[:, 0:ncols]
        P.op("dve", lambda e: e.memset(st[:, col:col + 1], 0.0), writes=[R["st"]])
        P.op("dve", lambda e: e.scalar_tensor_tensor(out=jk, in0=src_ap, scalar=1.0, in1=src_ap,
                                                     op0=ALU.mult, op1=ALU.mult, accum_out=st[:, col:col + 1]),
             reads=list(src_res), writes=[R["junk"], R["st"]], cost=0.2 + ncols / 900.0)
        P.op("dve", lambda e: e.tensor_scalar(out=st[:, col:col + 1], in0=st[:, col:col + 1],
                                              scalar1=1.0 / ncols, scalar2=RMS_EPS, op0=ALU.mult, op1=ALU.add),
             reads=[R["st"]], writes=[R["st"]])
        P.op("act", lambda e: e.activation(out=st[:, col:col + 1], in_=st[:, col:col + 1], func=AF.Sqrt),
             reads=[R["st"]], writes=[R["st"]])
        P.op("dve", lambda e: e.reciprocal(out=st[:, col:col + 1], in_=st[:, col:col + 1]),
             reads=[R["st"]], writes=[R["st"]])

    def transposes8(src_t, src_res, ptile, pname):
        for c in range(8):
            P.op("pe", lambda e, c=c: e.transpose(out=ptile[:, c, :], in_=src_t[:, c * 128:(c + 1) * 128],
                                                  identity=ident_b[:]),
                 reads=[src_res, R["ident_b"]], writes=[R[pname]])

    def _main_body(chk):
        for b in range(bpc):
            chk(0)
            P.barrier()
            for i in range(NT):
                xti = xt[i % 2]
                xr = f"xt{i % 2}"
                P.dma("sp", lambda e, xti=xti, b=b, i=i: e.dma_start(out=xti[:], in_=x_d[b, i * 128:(i + 1) * 128, :]),
                      writes=[R[xr]])
                rms_stats(xti[:], [R[xr]], D, 0)
                P.op("act", lambda e, xti=xti: e.activation(out=xb[:], in_=xti[:], func=AF.Copy, scale=st[:, 0:1]),
                     reads=[R[xr], R["st"]], writes=[R["xb"]])
                pt, pn = (pT0, "pT0") if i % 2 == 0 else (pT1, "pT1")
                transposes8(xb, R["xb"], pt, pn)
                P.op("dve", lambda e, pt=pt, i=i: e.tensor_tensor(
                    out=hT[:, :, i * 128:(i + 1) * 128], in0=pt[:],
                    in1=gmix[:].unsqueeze(2).to_broadcast([128, 8, 128]), op=ALU.mult),
                    reads=[R[pn], R["gmix"]], writes=[R["hT"]])

            chk(1)
            if not os.environ.get('KNOMEMSET'):
                P.op("pool", lambda e: e.memset(dv[:], 1.0), writes=[R["dv"]])
            for h in range(4):
                tok_cols = [512 + h * 128, 1024 + h * 128, 1536 + h * 128, 3072 + h * 128]
                feat_cols = [h * 128, 512 + h * 128, 2048 + h * 128, 2560 + h * 128]
                for k, c0 in enumerate(tok_cols):
                    P.dma("pool", lambda e, k=k, c0=c0: e.dma_start(
                        out=wtok[:, :, k * 128:(k + 1) * 128],
                        in_=w_in_d[:, c0:c0 + 128].rearrange("(c p) n -> p c n", p=128)), writes=[R["wtok"]])
                for k, c0 in enumerate(feat_cols):
                    P.dma("pool", lambda e, k=k, c0=c0: e.dma_start(
                        out=wfeat[:, :, k * 128:(k + 1) * 128],
                        in_=w_in_d[:, c0:c0 + 128].rearrange("(c p) n -> p c n", p=128)), writes=[R["wfeat"]])
                fdst = [(qTr, "qTr", 1.0), (kTr, "kTr", 1.0), (qTd, "qTd", 0.125), (kTd, "kTd", 1.0)]
                cnt = 0
                for k, (dst, dn, scl) in enumerate(fdst):
                    for tb in range(4):
                        pp, pn = (pA, "pA") if cnt % 2 == 0 else (pB, "pB")
                        cnt += 1
                        for c in range(8):
                            P.op("pe", lambda e, pp=pp, k=k, c=c, tb=tb: e.matmul(
                                out=pp[:], lhsT=wfeat[:, c, k * 128:(k + 1) * 128], rhs=hT[:, c, tb * 512:(tb + 1) * 512],
                                start=(c == 0), stop=(c == 7)),
                                reads=[R["wfeat"], R["hT"]], writes=[R[pn]])
                        P.op("act", lambda e, pp=pp, dst=dst, tb=tb, scl=scl: e.activation(
                            out=dst[:, tb * 512:(tb + 1) * 512], in_=pp[:], func=AF.Copy, scale=scl),
                            reads=[R[pn]], writes=[R[dn]])
                P.op("dve", lambda e, h=h: e.tensor_tensor(
                    out=qTrc[:].rearrange("p (n i) -> p n i", i=128), in0=qTr[:].rearrange("p (n i) -> p n i", i=128),
                    in1=cross_sb[:, h:h + 1, :].to_broadcast([128, NT, 128]), op=ALU.mult),
                    reads=[R["qTr"], R["cross"]], writes=[R["qTrc"]])
                chk(2)
                for ti in range(NT):
                    pp, pn = (pA, "pA") if ti % 2 == 0 else (pB, "pB")
                    for c in range(8):
                        P.op("pe", lambda e, pp=pp, c=c, ti=ti: e.matmul(
                            out=pp[:], lhsT=hT[:, c, ti * 128:(ti + 1) * 128], rhs=wtok[:, c, :],
                            start=(c == 0), stop=(c == 7)),
                            reads=[R["wtok"], R["hT"]], writes=[R[pn]])
                    KT = int(os.environ.get('KTOK', '15'))
                    if KT & 1:
                        P.op("dve", lambda e, pp=pp, ti=ti, h=h: e.tensor_scalar(
                            out=kd[:, ti, :], in0=pp[:, 0:128], scalar1=kdec_sb[:, h:h + 1], scalar2=None, op0=ALU.mult),
                            reads=[R[pn], R["kdec"]], writes=[R["kd"]])
                    if KT & 2:
                        P.op("act", lambda e, pp=pp, ti=ti: e.activation(out=rv[:, ti, :], in_=pp[:, 128:256], func=AF.Copy, scale=1.0),
                             reads=[R[pn]], writes=[R["rv"]])
                    if KT & 4:
                        P.op("act", lambda e, pp=pp, ti=ti: e.activation(out=sg[:, ti, :], in_=pp[:, 256:384], func=(AF.Copy if os.environ.get('KNOSILU') else AF.Silu)),
                             reads=[R[pn]], writes=[R["sg"]])
                    if KT & 8:
                        P.op("dve", lambda e, pp=pp, ti=ti: e.tensor_copy(out=dv[:, ti, 0:128], in_=pp[:, 384:512]),
                             reads=[R[pn]], writes=[R["dv"]])
                chk(3)
                for n in range(NT):
                    sl = slice(n * 128, (n + 1) * 128)
                    sb_i = n % 2
                    P.op("pe", lambda e, sl=sl: e.matmul(out=pC[:, 0:128], lhsT=kTr[:, sl], rhs=qTr[:, sl],
                                                          start=True, stop=True),
                         reads=[R["kTr"], R["qTr"]], writes=[R["pC"]])
                    P.op("dve", lambda e, sb_i=sb_i, h=h: e.tensor_tensor(out=stT[sb_i][:, 0:128], in0=pC[:, 0:128],
                                                                          in1=DT_sb[:, h, :], op=ALU.mult),
                         reads=[R["pC"], R["DT"]], writes=[R[f"stT{sb_i}"]])
                    P.op("pe", lambda e, sb_i=sb_i, n=n: e.matmul(out=pD[:, 0:128], lhsT=stT[sb_i][:, 0:128], rhs=rv[:, n, :],
                                                                  start=True, stop=(n == 0)),
                         reads=[R[f"stT{sb_i}"], R["rv"]], writes=[R["pD"]])
                    if n > 0:
                        P.op("pe", lambda e, sl=sl: e.matmul(out=pD[:, 0:128], lhsT=qTrc[:, sl], rhs=S_b[:],
                                                              start=False, stop=True),
                             reads=[R["qTrc"], R["S_b"]], writes=[R["pD"]])
                    P.op("act", lambda e: e.activation(out=o_sb[:], in_=pD[:, 0:128], func=AF.Copy),
                         reads=[R["pD"]], writes=[R["o_sb"]])
                    rms_stats(o_sb[:], [R["o_sb"]], 128, 1)
                    P.op("dve", lambda e, n=n, h=h: e.scalar_tensor_tensor(
                        out=cat[:, n, h * 128:(h + 1) * 128], in0=o_sb[:], scalar=st[:, 1:2], in1=sg[:, n, :],
                        op0=ALU.mult, op1=ALU.mult),
                        reads=[R["o_sb"], R["st"], R["sg"]], writes=[R["cat"]])
                    if n < NT - 1:
                        P.op("pe", lambda e, n=n: e.matmul(out=pA[:, 0:128], lhsT=kd[:, n, :], rhs=rv[:, n, :],
                                                            start=True, stop=True),
                             reads=[R["kd"], R["rv"]], writes=[R["pA"]])
                        if n == 0:
                            P.op("dve", lambda e: e.tensor_copy(out=S_f[:], in_=pA[:, 0:128]),
                                 reads=[R["pA"]], writes=[R["S_f"]])
                        else:
                            P.op("dve", lambda e, h=h: e.scalar_tensor_tensor(
                                out=S_f[:], in0=S_f[:], scalar=cdec[h], in1=pA[:, 0:128], op0=ALU.mult, op1=ALU.add),
                                reads=[R["pA"], R["S_f"]], writes=[R["S_f"]])
                        P.op("act", lambda e: e.activation(out=S_b[:], in_=S_f[:], func=AF.Copy),
                             reads=[R["S_f"]], writes=[R["S_b"]])
                chk(4)
                def combine(I, po0, pn0, po1, pn1, h=h):
                    P.op("dve", lambda e: e.reciprocal(out=st[:, 4:5], in_=po0[:, 128:129]), reads=[R[pn0]], writes=[R["st"]])
                    P.op("dve", lambda e: e.reciprocal(out=st[:, 5:6], in_=po1[:, 128:129]), reads=[R[pn1]], writes=[R["st"]])
                    P.op("dve", lambda e: e.tensor_tensor(out=st[:, 5:6], in0=st[:, 5:6], in1=neglam, op=ALU.mult),
                         reads=[R["st"], R["lamt"]], writes=[R["st"]])
                    P.op("dve", lambda e: e.tensor_scalar(out=t1[:], in0=po1[:, 0:128], scalar1=st[:, 5:6], scalar2=None,
                                                          op0=ALU.mult), reads=[R[pn1], R["st"]], writes=[R["t1"]])
                    P.op("dve", lambda e: e.scalar_tensor_tensor(out=o_sb[:], in0=po0[:, 0:128], scalar=st[:, 4:5], in1=t1[:],
                                                                 op0=ALU.mult, op1=ALU.add),
                         reads=[R[pn0], R["st"], R["t1"]], writes=[R["o_sb"]])
                    rms_stats(o_sb[:], [R["o_sb"]], 128, 6)
                    P.op("dve", lambda e: e.tensor_scalar(out=st[:, 6:7], in0=st[:, 6:7], scalar1=1.0 - LAMBDA_INIT,
                                                          scalar2=None, op0=ALU.mult), reads=[R["st"]], writes=[R["st"]])
                    P.op("dve", lambda e, I=I, h=h: e.scalar_tensor_tensor(
                        out=cat[:, I, 512 + h * 128:512 + (h + 1) * 128], in0=o_sb[:], scalar=st[:, 6:7], in1=gsub[:],
                        op0=ALU.mult, op1=ALU.mult),
                        reads=[R["o_sb"], R["st"], R["gsub"]], writes=[R["cat"]])


                items = [(I, J) for I in range(NT) for J in range(I + 1)]

                def acc(I):
                    return ((pE, "pE", pF, "pF") if I % 2 == 0 else (pA, "pA", pB, "pB"))

                def emit_S(k):
                    I, J = items[k]
                    isl = slice(I * 128, (I + 1) * 128)
                    jsl = slice(J * 128, (J + 1) * 128)
                    if k % 2 == 0:
                        pair, pns = pCD, ("pC", "pD")
                    else:
                        pair, pns = pT01f, ("pT0", "pT1")
                    sb_i = k % 3
                    for m in range(2):
                        msl = slice(m * 64, (m + 1) * 64)
                        P.op("pe", lambda e, pair=pair, msl=msl, jsl=jsl, isl=isl, m=m: e.matmul(
                            out=pair[:, m * 512:m * 512 + 128], lhsT=kTd[msl, jsl], rhs=qTd[msl, isl], start=True, stop=True),
                            reads=[R["kTd"], R["qTd"]], writes=[R[pns[m]]])
                    P.op("act", lambda e, pair=pair, sb_i=sb_i, h=h, dl_=I - J: e.activation(
                        out=stT[sb_i][:].rearrange("p (m i) -> p m i", m=2),
                        in_=pair[:].rearrange("p (m c) -> p m c", m=2)[:, :, 0:128],
                        func=AF.Exp, bias=bias_sb[:, h, dl_:dl_ + 1], scale=1.0),
                        reads=[R[pns[0]], R[pns[1]], R["bias"]], writes=[R[f"stT{sb_i}"]])
                    if J == I:
                        P.op("pool", lambda e, sb_i=sb_i: e.tensor_tensor(
                            out=stT[sb_i][:].rearrange("p (m i) -> p m i", m=2),
                            in0=stT[sb_i][:].rearrange("p (m i) -> p m i", m=2),
                            in1=mask_b[:].unsqueeze(1).to_broadcast([128, 2, 128]), op=ALU.mult),
                            reads=[R[f"stT{sb_i}"], R["mask_b"]], writes=[R[f"stT{sb_i}"]])

                def emit_PV(k):
                    I, J = items[k]
                    sb_i = k % 3
                    po0, pn0, po1, pn1 = acc(I)
                    for m, (po, pon) in enumerate(((po0, pn0), (po1, pn1))):
                        P.op("pe", lambda e, po=po, sb_i=sb_i, J=J, I=I, m=m: e.matmul(
                            out=po[:, 0:129], lhsT=stT[sb_i][:, m * 128:(m + 1) * 128], rhs=dv[:, J, 0:129],
                            start=(J == 0), stop=(J == I)),
                            reads=[R[f"stT{sb_i}"], R["dv"]], writes=[R[pon]])
                    if J == I:
                        combine(I, po0, pn0, po1, pn1)

                emit_S(0)
                for k in range(1, len(items)):
                    emit_S(k)
                    emit_PV(k - 1)
                emit_PV(len(items) - 1)
            chk(5)
            P.barrier()
            for _k in range(16):
                for _n in ("sc", "top", "itop"):
                    if f"{_n}_{_k}" not in R:
                        R[f"{_n}_{_k}"] = P.res(f"{_n}_{_k}")
            for _k in range(8):
                for _n in ("cand", "ctop", "cidx"):
                    if f"{_n}_{_k}" not in R:
                        R[f"{_n}_{_k}"] = P.res(f"{_n}_{_k}")
            SC = [R[f"sc_{k}"] for k in range(16)]
            TOP = [R[f"top_{k}"] for k in range(16)]
            ITOP = [R[f"itop_{k}"] for k in range(16)]
            CAND = [R[f"cand_{k}"] for k in range(8)]
            CTOP = [R[f"ctop_{k}"] for k in range(8)]
            CIDX = [R[f"cidx_{k}"] for k in range(8)]
            def front(i):
                par = i % 2
                x1, xn, eidx, gate = x1s[par], xns[par], eidxs[par], gates[par]
                x1n, xnn, eidxn, gaten = f"x1_{par}", f"xn_{par}", f"eidx_{par}", f"gate_{par}"
                xnb, xnbn = xnbs[par], f"xnb_{par}"
                rows = slice(i * 128, (i + 1) * 128)
                xti = xt[i % 2]
                xr = f"xt{i % 2}"
                P.dma("sp", lambda e, xti=xti, b=b, rows=rows: e.dma_start(out=xti[:], in_=x_d[b, rows, :]),
                      writes=[R[xr]])
                transposes8(cat[:, i, :], R["cat"], pT0, "pT0")
                P.op("act", lambda e: e.activation(out=catT[:], in_=pT0[:], func=AF.Copy), reads=[R["pT0"]], writes=[R["catT"]])
                for half, (pp, pn) in enumerate(((pA, "pA"), (pB, "pB"))):
                    for c in range(8):
                        P.op("pe", lambda e, pp=pp, c=c, half=half: e.matmul(
                            out=pp[:], lhsT=catT[:, c, :], rhs=w_out_b[:, c, half * 512:(half + 1) * 512],
                            start=(c == 0), stop=(c == 7)),
                            reads=[R["catT"], R["w_out_b"]], writes=[R[pn]])
                    P.op("dve", lambda e, pp=pp, half=half, xti=xti: e.tensor_tensor(
                        out=x1[:, half * 512:(half + 1) * 512], in0=pp[:], in1=xti[:, half * 512:(half + 1) * 512], op=ALU.add),
                        reads=[R[pn], R[xr]], writes=[R[x1n]], cost=0.7)
                if debug:
                    P.dma("sp", lambda e, b=b, rows=rows: e.dma_start(out=dbg_d[b, rows, :], in_=x1[:]),
                          reads=[R[x1n]], chan=R[x1n])
                rms_stats(x1[:], [R[x1n]], D, 8)
                P.op("dve", lambda e: e.scalar_tensor_tensor(out=xnb[:], in0=x1[:], scalar=st[:, 8:9], in1=gffn[:],
                                                             op0=ALU.mult, op1=ALU.mult),
                     reads=[R[x1n], R["st"], R["gffn"]], writes=[R[xnbn]], cost=1.2)
                transposes8(xnb, R[xnbn], pT1, "pT1")
                P.op("act", lambda e: e.activation(out=xnT[:], in_=pT1[:], func=AF.Copy), reads=[R["pT1"]], writes=[R["xnT"]])
                for g4 in range(4):
                    pp, pn = (pC, "pC") if g4 % 2 == 0 else (pD, "pD")
                    for q4 in range(4):
                        cb = g4 * 4 + q4
                        for c in range(8):
                            P.op("pe", lambda e, pp=pp, q4=q4, cb=cb, c=c: e.matmul(
                                out=pp[:, q4 * 128:(q4 + 1) * 128], lhsT=wq_b[:, c, cb * 128:(cb + 1) * 128], rhs=xnT[:, c, :],
                                start=(c == 0), stop=(c == 7)),
                                reads=[R["wq_b"], R["xnT"]], writes=[R[pn]])
                    P.op("act", lambda e, pp=pp, g4=g4: e.activation(
                        out=qT_sb[:, g4 * 4:(g4 + 1) * 4, :], in_=pp[:].rearrange("p (a b) -> p a b", b=128), func=AF.Copy),
                        reads=[R[pn]], writes=[R["qT_sb"]])
                for g4 in range(4):
                    pp, pn = ((pA, "pA"), (pB, "pB"), (pC, "pC"), (pD, "pD"))[g4]
                    for q4 in range(4):
                        cb = g4 * 4 + q4
                        P.op("pe", lambda e, pp=pp, q4=q4, cb=cb: e.matmul(
                            out=pp[:, q4 * 128:(q4 + 1) * 128], lhsT=qT_sb[:, cb, :], rhs=skT_b[:, cb, :], start=True, stop=True),
                            reads=[R["qT_sb"], R["skT_b"]], writes=[R[pn]])
                    P.op("act", lambda e, pp=pp, g4=g4: e.activation(
                        out=sc[:, g4 * 4:(g4 + 1) * 4, :], in_=pp[:].rearrange("p (a b) -> p a b", b=128), func=AF.Copy),
                        reads=[R[pn]], writes=SC[g4 * 4:(g4 + 1) * 4])
                for cb in range(16):
                    P.op("dve", lambda e, cb=cb: e.max(out=top[:, cb, 0:8], in_=sc[:, cb, :]), reads=[SC[cb]], writes=[TOP[cb]])
                for cb in range(16):
                    P.op("dve", lambda e, cb=cb: e.max_index(out=itop[:, cb, 0:8], in_max=top[:, cb, 0:8], in_values=sc[:, cb, :]),
                         reads=[SC[cb], TOP[cb]], writes=[ITOP[cb]])
                for cb in range(16):
                    P.op("dve", lambda e, cb=cb: e.match_replace(out=sc[:, cb, :], in_to_replace=top[:, cb, 0:8],
                                                                 in_values=sc[:, cb, :], imm_value=-1e30),
                         reads=[TOP[cb]], writes=[SC[cb]])
                for cb in range(16):
                    P.op("dve", lambda e, cb=cb: e.max(out=top[:, cb, 8:16], in_=sc[:, cb, :]), reads=[SC[cb]], writes=[TOP[cb]])
                for cb in range(16):
                    P.op("dve", lambda e, cb=cb: e.max_index(out=itop[:, cb, 8:16], in_max=top[:, cb, 8:16], in_values=sc[:, cb, :]),
                         reads=[SC[cb], TOP[cb]], writes=[ITOP[cb]])
                P.op("dve", lambda e: e.tensor_copy(out=itopf[:], in_=itop[:]), reads=ITOP, writes=[R["itopf"]])
                top4 = top[:].rearrange("p (h two) r -> p h two r", two=2)
                itf4 = itopf[:].rearrange("p (h two) r -> p h two r", two=2)
                P.op("dve", lambda e: e.tensor_tensor(
                    out=cand[:].rearrange("p h (a b) -> p h a b", b=16),
                    in0=top4[:, :, 0, :].unsqueeze(3).to_broadcast([128, 8, 16, 16]),
                    in1=top4[:, :, 1, :].unsqueeze(2).to_broadcast([128, 8, 16, 16]), op=ALU.add),
                    reads=TOP, writes=CAND, cost=2.3)
                for hh in range(8):
                    P.op("dve", lambda e, hh=hh: e.max(out=ctop[:, hh, 0:8], in_=cand[:, hh, :]), reads=[CAND[hh]], writes=[CTOP[hh]])
                for hh in range(8):
                    P.op("dve", lambda e, hh=hh: e.max_index(out=cidx[:, hh, 0:8], in_max=ctop[:, hh, 0:8], in_values=cand[:, hh, :]),
                         reads=[CAND[hh], CTOP[hh]], writes=[CIDX[hh]])
                for hh in range(8):
                    P.op("dve", lambda e, hh=hh: e.match_replace(out=cand[:, hh, :], in_to_replace=ctop[:, hh, 0:8],
                                                                 in_values=cand[:, hh, :], imm_value=-1e30),
                         reads=[CTOP[hh]], writes=[CAND[hh]])
                for hh in range(8):
                    P.op("dve", lambda e, hh=hh: e.max(out=ctop[:, hh, 8:16], in_=cand[:, hh, :]), reads=[CAND[hh]], writes=[CTOP[hh]])
                for hh in range(8):
                    P.op("dve", lambda e, hh=hh: e.max_index(out=cidx[:, hh, 8:16], in_max=ctop[:, hh, 8:16], in_values=cand[:, hh, :]),
                         reads=[CAND[hh], CTOP[hh]], writes=[CIDX[hh]])
                P.op("dve", lambda e: e.tensor_copy(out=cidxf[:], in_=cidx[:].rearrange("p h k -> p (h k)")),
                     reads=CIDX, writes=[R["cidxf"]])
                P.op("dve", lambda e: e.tensor_tensor(
                    out=eq[:].rearrange("p h (k r) -> p (h k) r", r=16),
                    in0=cidxf[:].unsqueeze(2).to_broadcast([128, 128, 16]),
                    in1=thr[:].unsqueeze(1).to_broadcast([128, 128, 16]), op=ALU.is_ge),
                    reads=[R["cidxf"], R["thr"]], writes=CAND, cost=2.3)
                P.op("dve", lambda e: e.tensor_reduce(
                    out=r12f[:, 0, :], in_=eq[:].rearrange("p h (k r) -> p (h k) r", r=16), axis=AX.X, op=ALU.add),
                    reads=CAND, writes=[R["r12f"]], cost=2.3)
                P.op("dve", lambda e: e.scalar_tensor_tensor(out=r12f[:, 1, :], in0=r12f[:, 0, :], scalar=-16.0, in1=cidxf[:],
                                                             op0=ALU.mult, op1=ALU.add),
                     reads=[R["r12f"], R["cidxf"]], writes=[R["r12f"]])
                for w in range(2):
                    P.op("dve", lambda e, w=w: e.tensor_tensor(
                        out=eq[:].rearrange("p h (k r) -> p h k r", r=16),
                        in0=iota16[:].unsqueeze(1).unsqueeze(1).to_broadcast([128, 8, 16, 16]),
                        in1=r12f[:, w, :].rearrange("p (h k) -> p h k", k=16).unsqueeze(3).to_broadcast([128, 8, 16, 16]),
                        op=ALU.is_equal), reads=[R["iota16"], R["r12f"]], writes=CAND, cost=2.3)
                    P.op("dve", lambda e, w=w: e.tensor_tensor(
                        out=eq[:].rearrange("p h (k r) -> p h k r", r=16),
                        in0=eq[:].rearrange("p h (k r) -> p h k r", r=16),
                        in1=itf4[:, :, w, :].unsqueeze(2).to_broadcast([128, 8, 16, 16]), op=ALU.mult),
                        reads=[R["itopf"]], writes=CAND, cost=2.3)
                    P.op("dve", lambda e, w=w: e.tensor_reduce(
                        out=i12f[:, w, :], in_=eq[:].rearrange("p h (k r) -> p (h k) r", r=16), axis=AX.X, op=ALU.add),
                        reads=CAND, writes=[R["i12f"]], cost=2.3)
                P.op("dve", lambda e: e.scalar_tensor_tensor(out=ef[:], in0=i12f[:, 0, :], scalar=128.0, in1=i12f[:, 1, :],
                                                             op0=ALU.mult, op1=ALU.add), reads=[R["i12f"]], writes=[R["ef"]])
                P.op("dve", lambda e: e.tensor_scalar(out=ef[:], in0=ef[:], scalar1=0.0, scalar2=16383.0, op0=ALU.max, op1=ALU.min),
                 reads=[R["ef"]], writes=[R["ef"]])
                P.op("dve", lambda e: e.tensor_copy(out=eidx[:], in_=ef[:]), reads=[R["ef"]], writes=[R[eidxn]])
                P.op("dve", lambda e: e.tensor_scalar(out=gz[:, 0:8], in0=ctop[:, :, 0], scalar1=-1.0, scalar2=None, op0=ALU.mult),
                     reads=CTOP, writes=[R["gz"]])
                for hh in range(8):
                    P.op("act", lambda e, hh=hh: e.activation(out=gate[:, hh * 16:(hh + 1) * 16], in_=ctop[:, hh, :], func=AF.Exp,
                                                              bias=gz[:, hh:hh + 1], scale=1.0),
                         reads=[CTOP[hh], R["gz"]], writes=[R[gaten]])
                P.op("dve", lambda e: e.tensor_reduce(out=gz[:, 8:16], in_=gate[:].rearrange("p (h k) -> p h k", k=16),
                                                      axis=AX.X, op=ALU.add), reads=[R[gaten]], writes=[R["gz"]])
                P.op("dve", lambda e: e.reciprocal(out=gz[:, 8:16], in_=gz[:, 8:16]), reads=[R["gz"]], writes=[R["gz"]])
                P.op("dve", lambda e: e.tensor_tensor(
                    out=gate[:].rearrange("p (h k) -> p h k", k=16), in0=gate[:].rearrange("p (h k) -> p h k", k=16),
                    in1=gz[:, 8:16].unsqueeze(2).to_broadcast([128, 8, 16]), op=ALU.mult),
                    reads=[R[gaten], R["gz"]], writes=[R[gaten]])

            def stream(i, filler):
                par = i % 2
                x1, xn, eidx, gate = x1s[par], xns[par], eidxs[par], gates[par]
                x1n, xnn, eidxn, gaten = f"x1_{par}", f"xn_{par}", f"eidx_{par}", f"gate_{par}"
                xnb, xnbn = xnbs[par], f"xnb_{par}"
                rows = slice(i * 128, (i + 1) * 128)
                P.op("dve", lambda e: e.memset(actv[:], 0.0), writes=[R[f"actv{i}"] for i in range(8)])
                scr = [R["scr0"], R["scr1"], R["scr2"], R["scr3"]]
                for s in range(128):
                    gb = s % NG
                    di = s % 2
                    s8 = s % 8
                    P.dma("pool", lambda e, s=s, gb=gb: e.indirect_dma_start(
                        out=gbuf[gb][:], out_offset=None, in_=uv_d,
                        in_offset=bass.IndirectOffsetOnAxis(ap=eidx[:, s:s + 1], axis=0)),
                        reads=[R[eidxn]] + scr, writes=[R[f"gbuf{gb}"]])
                    P.op("dve", lambda e, s=s, gb=gb, di=di: e.scalar_tensor_tensor(
                        out=jkb[di][:], in0=gbuf[gb][:, 0:D], scalar=1.0, in1=xnb[:], op0=ALU.mult, op1=ALU.mult,
                        accum_out=actv[:, s:s + 1]),
                        reads=[R[f"gbuf{gb}"], R[xnbn]], writes=[R[f"actv{s8}"]])
                    P.op("act", lambda e, s=s: e.activation(out=cf[:, s:s + 1], in_=actv[:, s:s + 1], func=AF.Gelu),
                         reads=[R[f"actv{s8}"]], writes=[R[f"cf{s8}"]])
                    P.op("act", lambda e, s=s: e.activation(out=cf2[:, s:s + 1], in_=cf[:, s:s + 1], func=AF.Copy,
                                                            scale=gate[:, s:s + 1]),
                         reads=[R[f"cf{s8}"], R[gaten]], writes=[R[f"cf{s8}"]])
                    P.op("act", lambda e, s=s, di=di: e.activation(out=dg[di][:], in_=ident_f[:], func=AF.Copy,
                                                                   scale=cf2[:, s:s + 1]),
                         reads=[R["ident_f"], R[f"cf{s8}"]], writes=[R[f"dg{di}"]])
                    for half, (pp, pn) in enumerate(((pE, "pE"), (pF, "pF"))):
                        P.op("pe", lambda e, pp=pp, di=di, gb=gb, half=half, s=s: e.matmul(
                            out=pp[:], lhsT=dg[di][:], rhs=gbuf[gb][:, D + half * 512:D + (half + 1) * 512],
                            start=(s == 0), stop=(s == 127)),
                            reads=[R[f"dg{di}"], R[f"gbuf{gb}"]], writes=[R[pn]])
                    filler()

            def tail(i):
                par = i % 2
                x1, xn, eidx, gate = x1s[par], xns[par], eidxs[par], gates[par]
                x1n, xnn, eidxn, gaten = f"x1_{par}", f"xn_{par}", f"eidx_{par}", f"gate_{par}"
                xnb, xnbn = xnbs[par], f"xnb_{par}"
                rows = slice(i * 128, (i + 1) * 128)
                for half, (pp, pn) in enumerate(((pE, "pE"), (pF, "pF"))):
                    P.op("dve", lambda e, pp=pp, half=half: e.tensor_tensor(
                        out=x2[:, half * 512:(half + 1) * 512], in0=pp[:], in1=x1[:, half * 512:(half + 1) * 512], op=ALU.add),
                        reads=[R[pn], R[x1n]], writes=[R["x2"]])
                rms_stats(x2[:], [R["x2"]], D, 9)
                P.op("dve", lambda e: e.scalar_tensor_tensor(out=ob[:], in0=x2[:], scalar=st[:, 9:10], in1=gfin[:],
                                                         op0=ALU.mult, op1=ALU.mult),
                 reads=[R["st"], R["gfin"]], writes=[R["x2"]])
                P.dma("sp", lambda e, b=b, rows=rows: e.dma_start(out=out_d[b, rows, :], in_=ob[:]),
                  reads=[R["ob"]], chan=R["ob"])

            P.capture = []
            front(0)
            cap = P.capture
            P.capture = None
            P.replay(cap)
            for i in range(NT):
                if i + 1 < NT:
                    P.capture = []
                    front(i + 1)
                    cap = P.capture
                    P.capture = None
                else:
                    cap = []
                pos = [0]
                tot = sum((it[5] or 0.0) for it in cap if it[0] == "op") + 1e-6
                budget = tot / 116.0
                spent = [0.0]
                slot = [0]

                def filler(cap=cap, pos=pos, budget=budget, spent=spent, slot=slot):
                    slot[0] += 1
                    target = budget * slot[0]
                    j = pos[0]
                    n0 = j
                    while j < len(cap) and (spent[0] < target) and (j - n0) < 24:
                        if cap[j][0] == "op":
                            spent[0] += (cap[j][5] or 0.0)
                        j += 1
                    P.replay(cap[pos[0]:j])
                    pos[0] = j

                stream(i, filler)
                P.replay(cap[pos[0]:])
                tail(i)


    def chk(level):
        if stop == level:
            raise _Stop()

    try:
        _main_body(chk)
    except _Stop:
        pass
    fin = ([R["ob"]] if R["ob"].r else []) + ([R["x1"], R["x1_b"]] if debug else [])
    P.emit(final_waits=fin)
    P.close()
    return nc, consts


def make_in_maps(inputs, consts, bpc=BPC, ncores=NCORES):
    f = np.float32
    x = np.ascontiguousarray(inputs["x"], dtype=f)
    common = {
        "w_in": np.ascontiguousarray(inputs["w_in"][0]),
        "w_out": np.ascontiguousarray(inputs["w_out"][0]),
        "wq": np.ascontiguousarray(inputs["peer_wq"][0]),
        "skT": np.ascontiguousarray(np.transpose(inputs["peer_subkeys"][0].reshape(16, 128, 128), (2, 0, 1))),
        "u": np.ascontiguousarray(inputs["peer_u"][0]),
        "v": np.ascontiguousarray(inputs["peer_v"][0]),
        "gmix_col": np.ascontiguousarray(inputs["norm_mix_g"][0].reshape(8, 128).T),
        "gffn_b": np.ascontiguousarray(np.broadcast_to(inputs["norm_ffn_g"][0][None], (128, D))),
        "gfin_b": np.ascontiguousarray(np.broadcast_to(inputs["final_norm_g"][None], (128, D))),
        "gsub_b": np.ascontiguousarray(np.broadcast_to(inputs["diff_subln_g"][0][None], (128, 128))),
        "dl_b": np.ascontiguousarray(np.broadcast_to(inputs["diff_lambda"][0][None], (128, 4, 64))),
    }
    common = {k: np.asarray(a, dtype=f) for k, a in common.items()}
    common.update(consts)
    maps = []
    for c in range(ncores):
        m = dict(common)
        m["x"] = np.ascontiguousarray(x[c * bpc:(c + 1) * bpc])
        maps.append(m)
    return maps


_CACHE = {}


def kernel(**inputs):
    inputs = {k: np.asarray(v) for k, v in inputs.items()}
    if "nc" not in _CACHE:
        _CACHE["nc"] = build_program()
    nc, consts = _CACHE["nc"]
    in_maps = make_in_maps(inputs, consts)
    res = run_bass_kernel_spmd(nc, in_maps, core_ids=list(range(NCORES)))
    out = np.concatenate([r["out"] for r in res.results], axis=0)
    return out.astype(np.float32)
```
